# Optimizing a Trainium2 kernel written in Bass

```python
import math
import jax
import jax.numpy as jnp
from jax import lax
import numpy as np


D_MODEL = 2048
BATCH = 8
SEQ = 2048
DEPTH = 4

GRID_W = 64
CTX_LEN = 256
HEAD_DIM = 64
BRANCH_W = 512
N_BRANCH = 4
S5_CH = 16
S5_GROUPS = BRANCH_W // S5_CH
S5_STATE = 64
WIN_HEADS = BRANCH_W // HEAD_DIM
WIN_KV_HEADS = 2
WINDOW = 128
BLOCK = 128
DIFF_HEADS = BRANCH_W // (2 * HEAD_DIM)
DIFF_VDIM = 2 * HEAD_DIM
NA_HEADS = BRANCH_W // HEAD_DIM
NA_WIN_R = 8
NA_WIN_C = 16
N_GROUPS = 4
EXPERTS_PER_GROUP = 4
N_EXPERTS = N_GROUPS * EXPERTS_PER_GROUP
TOP_K_IN_GROUP = 2
EXPERT_FF = D_MODEL // 2
ROPE_BASE = 100.0
EPS = 1e-6
NEG_INF = -1e30
IN_SIZES = (BRANCH_W,
            WIN_HEADS * HEAD_DIM, WIN_KV_HEADS * HEAD_DIM, WIN_KV_HEADS * HEAD_DIM,
            2 * DIFF_HEADS * HEAD_DIM, 2 * DIFF_HEADS * HEAD_DIM, DIFF_HEADS * DIFF_VDIM,
            NA_HEADS * HEAD_DIM, NA_HEADS * HEAD_DIM, NA_HEADS * HEAD_DIM,
            N_BRANCH * D_MODEL)
IN_WIDTH = sum(IN_SIZES)

kernel_name = 'hybrid_prefix_dit_block'


def rms_norm(x, gain):
    xf = x.astype(jnp.float32)
    y = xf * lax.rsqrt(jnp.mean(xf * xf, axis=-1, keepdims=True) + EPS)
    return (y * gain.astype(jnp.float32)).astype(x.dtype)


def softmax_f32(s):
    return jax.nn.softmax(s.astype(jnp.float32), axis=-1)


def axial_rope_tables(n_tokens):
    t = jnp.arange(n_tokens, dtype=jnp.int32)
    row = (t // GRID_W).astype(jnp.float32)
    col = (t % GRID_W).astype(jnp.float32)
    per_axis = HEAD_DIM // 4
    inv_freq = ROPE_BASE ** (-jnp.arange(per_axis, dtype=jnp.float32) / per_axis)
    ang = jnp.concatenate([row[:, None] * inv_freq, col[:, None] * inv_freq], axis=-1)
    return jnp.cos(ang), jnp.sin(ang)


def apply_rope(x, cos, sin):
    shape = (x.shape[1],) + (1,) * (x.ndim - 3) + (HEAD_DIM // 2,)
    cos = cos.reshape(shape)
    sin = sin.reshape(shape)
    x1, x2 = jnp.split(x.astype(jnp.float32), 2, axis=-1)
    return jnp.concatenate([x1 * cos - x2 * sin, x1 * sin + x2 * cos], axis=-1).astype(x.dtype)


def _diag_combine(left, right):
    a1, b1 = left
    a2, b2 = right
    return a2 * a1, a2 * b1 + b2


def s5_branch(u, a_re, a_im, log_step, b_re, b_im, c_re, c_im, d_skip, w_glu, b_glu):
    bsz, tlen, _ = u.shape
    f32 = jnp.float32
    uf = u.astype(f32).reshape(bsz, tlen, S5_GROUPS, S5_CH)
    u_rev = jnp.concatenate([uf[:, :CTX_LEN][:, ::-1], uf[:, CTX_LEN:][:, ::-1]], axis=1)
    y = d_skip.astype(f32).reshape(S5_GROUPS, S5_CH) * uf
    for direction, seq in enumerate((uf, u_rev)):
        lam = lax.complex(a_re[direction].astype(f32), a_im[direction].astype(f32))
        dt = jnp.exp(log_step[direction].astype(f32))[:, None]
        lam_bar = jnp.exp(lam * dt)
        b_mat = lax.complex(b_re[direction].astype(f32), b_im[direction].astype(f32))
        b_bar = ((lam_bar - 1.0) / lam)[..., None] * b_mat
        c_mat = lax.complex(c_re[direction].astype(f32), c_im[direction].astype(f32))
        bu = jnp.einsum('gpi,btgi->btgp', b_bar, seq.astype(jnp.complex64))
        a_seq = jnp.broadcast_to(lam_bar, (1, tlen, S5_GROUPS, S5_STATE))
        _, states = lax.associative_scan(_diag_combine, (a_seq, bu), axis=1)
        yd = jnp.einsum('gip,btgp->btgi', c_mat, states).real
        if direction == 1:
            yd = jnp.concatenate([yd[:, :CTX_LEN][:, ::-1], yd[:, CTX_LEN:][:, ::-1]], axis=1)
        y = y + yd
    y = jax.nn.gelu(y.reshape(bsz, tlen, BRANCH_W))
    out = y * jax.nn.sigmoid(y @ w_glu.astype(f32) + b_glu.astype(f32))
    return out.astype(u.dtype)


def window_branch(q, k, v, q_gain, k_gain, sink, cos, sin):
    bsz, tlen, _ = q.shape
    n_lat = tlen - CTX_LEN
    grp = WIN_HEADS // WIN_KV_HEADS
    scale = HEAD_DIM ** -0.5
    q = rms_norm(q.reshape(bsz, tlen, WIN_HEADS, HEAD_DIM), q_gain)
    k = rms_norm(k.reshape(bsz, tlen, WIN_KV_HEADS, HEAD_DIM), k_gain)
    v = v.reshape(bsz, tlen, WIN_KV_HEADS, HEAD_DIM)
    qc, ql = q[:, :CTX_LEN], apply_rope(q[:, CTX_LEN:], cos, sin)
    kc, kl = k[:, :CTX_LEN], apply_rope(k[:, CTX_LEN:], cos, sin)
    vc, vl = v[:, :CTX_LEN], v[:, CTX_LEN:]
    sink_f = sink.astype(jnp.float32).reshape(WIN_KV_HEADS, grp)
    nb = n_lat // BLOCK
    n_shift = 1 + 2 * (WINDOW // BLOCK)
    span = n_shift * BLOCK
    pad = ((0, 0), (WINDOW, WINDOW), (0, 0), (0, 0))
    kp = jnp.pad(kl, pad)
    vp = jnp.pad(vl, pad)
    k_band = jnp.concatenate([kp[:, i * BLOCK:i * BLOCK + n_lat].reshape(bsz, nb, BLOCK, WIN_KV_HEADS, HEAD_DIM) for i in range(n_shift)], axis=2)
    v_band = jnp.concatenate([vp[:, i * BLOCK:i * BLOCK + n_lat].reshape(bsz, nb, BLOCK, WIN_KV_HEADS, HEAD_DIM) for i in range(n_shift)], axis=2)
    qb = ql.reshape(bsz, nb, BLOCK, WIN_KV_HEADS, grp, HEAD_DIM)
    s_loc = jnp.einsum('bnqkgd,bnskd->bnkgqs', qb, k_band).astype(jnp.float32) * scale
    s_ctx = jnp.einsum('bnqkgd,bskd->bnkgqs', qb, kc).astype(jnp.float32) * scale
    blk = jnp.arange(nb)[:, None] * BLOCK
    qpos = blk + jnp.arange(BLOCK)[None, :]
    kpos = blk - WINDOW + jnp.arange(span)[None, :]
    valid = ((jnp.abs(qpos[:, :, None] - kpos[:, None, :]) <= WINDOW)
             & (kpos[:, None, :] >= 0) & (kpos[:, None, :] < n_lat))
    s_loc = jnp.where(valid[None, :, None, None], s_loc, NEG_INF)
    sink_col = jnp.broadcast_to(sink_f[None, None, :, :, None, None], s_loc.shape[:-1] + (1,))
    p = softmax_f32(jnp.concatenate([s_loc, s_ctx, sink_col], axis=-1))
    p_loc = p[..., :span].astype(v.dtype)
    p_ctx = p[..., span:span + CTX_LEN].astype(v.dtype)
    o_lat = (jnp.einsum('bnkgqs,bnskd->bnqkgd', p_loc, v_band)
             + jnp.einsum('bnkgqs,bskd->bnqkgd', p_ctx, vc))
    o_lat = o_lat.reshape(bsz, n_lat, WIN_HEADS * HEAD_DIM)
    qcr = qc.reshape(bsz, CTX_LEN, WIN_KV_HEADS, grp, HEAD_DIM)
    s_cc = jnp.einsum('bqkgd,bskd->bkgqs', qcr, kc).astype(jnp.float32) * scale
    sink_cc = jnp.broadcast_to(sink_f[None, :, :, None, None], s_cc.shape[:-1] + (1,))
    p_cc = softmax_f32(jnp.concatenate([s_cc, sink_cc], axis=-1))[..., :CTX_LEN].astype(v.dtype)
    o_ctx = jnp.einsum('bkgqs,bskd->bqkgd', p_cc, vc).reshape(bsz, CTX_LEN, WIN_HEADS * HEAD_DIM)
    return jnp.concatenate([o_ctx, o_lat], axis=1)


def diff_branch(q, k, v, q_gain, k_gain, lam_params, sub_gain, lam_init, cos, sin):
    bsz, tlen, _ = q.shape
    n_lat = tlen - CTX_LEN
    scale = HEAD_DIM ** -0.5
    q = rms_norm(q.reshape(bsz, tlen, DIFF_HEADS, 2, HEAD_DIM), q_gain)
    k = rms_norm(k.reshape(bsz, tlen, DIFF_HEADS, 2, HEAD_DIM), k_gain)
    v = v.reshape(bsz, tlen, DIFF_HEADS, DIFF_VDIM)
    lp = lam_params.astype(jnp.float32)
    lam = jnp.exp(jnp.sum(lp[0] * lp[1])) - jnp.exp(jnp.sum(lp[2] * lp[3])) + lam_init
    qc, ql = q[:, :CTX_LEN], apply_rope(q[:, CTX_LEN:], cos, sin)
    kc = k[:, :CTX_LEN]
    k_all = jnp.concatenate([kc, apply_rope(k[:, CTX_LEN:], cos, sin)], axis=1)
    vc = v[:, :CTX_LEN]

    def diff_attend(q_blk, keys, values):
        s = jnp.einsum('bqhmd,bkhmd->bhmqk', q_blk, keys).astype(jnp.float32) * scale
        p = softmax_f32(s)
        p_diff = (p[:, :, 0] - lam * p[:, :, 1]).astype(values.dtype)
        return jnp.einsum('bhqk,bkhe->bqhe', p_diff, values)

    nb = n_lat // BLOCK
    q_blocks = jnp.moveaxis(ql.reshape(bsz, nb, BLOCK, DIFF_HEADS, 2, HEAD_DIM), 1, 0)
    o_lat = lax.map(lambda qb: diff_attend(qb, k_all, v), q_blocks)
    o_lat = jnp.moveaxis(o_lat, 0, 1).reshape(bsz, n_lat, DIFF_HEADS, DIFF_VDIM)
    o_ctx = diff_attend(qc, kc, vc)
    o = jnp.concatenate([o_ctx, o_lat], axis=1)
    o = rms_norm(o, sub_gain) * (1.0 - lam_init)
    return o.reshape(bsz, tlen, DIFF_HEADS * DIFF_VDIM)


def neighborhood_branch(q, k, v, q_gain, k_gain, rpb):
    bsz, tlen, _ = q.shape
    n_lat = tlen - CTX_LEN
    rows = n_lat // GRID_W
    win_r = min(NA_WIN_R, rows)
    span = win_r * GRID_W
    scale = HEAD_DIM ** -0.5
    q = rms_norm(q.reshape(bsz, tlen, NA_HEADS, HEAD_DIM), q_gain)
    k = rms_norm(k.reshape(bsz, tlen, NA_HEADS, HEAD_DIM), k_gain)
    v = v.reshape(bsz, tlen, NA_HEADS, HEAD_DIM)
    qc, kc, vc = q[:, :CTX_LEN], k[:, :CTX_LEN], v[:, :CTX_LEN]
    qg = q[:, CTX_LEN:].reshape(bsz, rows, GRID_W, NA_HEADS, HEAD_DIM)
    kg = k[:, CTX_LEN:].reshape(bsz, rows, GRID_W, NA_HEADS, HEAD_DIM)
    vg = v[:, CTX_LEN:].reshape(bsz, rows, GRID_W, NA_HEADS, HEAD_DIM)
    r = jnp.arange(rows)
    row_idx = jnp.clip(r - win_r // 2, 0, rows - win_r)[:, None] + jnp.arange(win_r)[None, :]
    k_rows = kg[:, row_idx].reshape(bsz, rows, span, NA_HEADS, HEAD_DIM)
    v_rows = vg[:, row_idx].reshape(bsz, rows, span, NA_HEADS, HEAD_DIM)
    col = jnp.arange(GRID_W)
    col_start = jnp.clip(col - NA_WIN_C // 2, 0, GRID_W - NA_WIN_C)
    col_ok = (col[None, :] >= col_start[:, None]) & (col[None, :] < col_start[:, None] + NA_WIN_C)
    mask = jnp.broadcast_to(col_ok[:, None, :], (GRID_W, win_r, GRID_W)).reshape(GRID_W, span)
    r_off = row_idx - r[:, None] + (NA_WIN_R - 1)
    c_off = jnp.clip(col[None, :] - col[:, None] + (NA_WIN_C - 1), 0, 2 * NA_WIN_C - 2)
    bias = rpb[:, r_off[:, None, :, None], c_off[None, :, None, :]]
    bias = jnp.moveaxis(bias.reshape(NA_HEADS, rows, GRID_W, span), 0, 1).astype(jnp.float32)
    s_loc = jnp.einsum('brqhd,brkhd->brhqk', qg, k_rows).astype(jnp.float32) * scale + bias[None]
    s_loc = jnp.where(mask[None, None, None], s_loc, NEG_INF)
    s_ctx = jnp.einsum('brqhd,bkhd->brhqk', qg, kc).astype(jnp.float32) * scale
    p = softmax_f32(jnp.concatenate([s_loc, s_ctx], axis=-1))
    o_lat = (jnp.einsum('brhqk,brkhd->brqhd', p[..., :span].astype(v.dtype), v_rows)
             + jnp.einsum('brhqk,bkhd->brqhd', p[..., span:].astype(v.dtype), vc))
    o_lat = o_lat.reshape(bsz, n_lat, NA_HEADS * HEAD_DIM)
    s_cc = jnp.einsum('bqhd,bkhd->bhqk', qc, kc).astype(jnp.float32) * scale
    o_ctx = jnp.einsum('bhqk,bkhd->bqhd', softmax_f32(s_cc).astype(v.dtype), vc).reshape(bsz, CTX_LEN, NA_HEADS * HEAD_DIM)
    return jnp.concatenate([o_ctx, o_lat], axis=1)


def hybrid_mixer(h, w_in, w_branch, w_out,
                 s5_a_re, s5_a_im, s5_log_step, s5_b_re, s5_b_im, s5_c_re, s5_c_im, s5_d, s5_w_glu, s5_b_glu,
                 win_qn, win_kn, win_sink, diff_qn, diff_kn, diff_lambda, diff_subln, lam_init,
                 na_qn, na_kn, na_rpb, cos, sin):
    offs = []
    acc = 0
    for size in IN_SIZES[:-1]:
        acc += size
        offs.append(acc)
    u_a, q_b, k_b, v_b, q_c, k_c, v_c, q_d, k_d, v_d, gates = jnp.split(h @ w_in, offs, axis=-1)
    y_a = s5_branch(u_a, s5_a_re, s5_a_im, s5_log_step, s5_b_re, s5_b_im, s5_c_re, s5_c_im, s5_d, s5_w_glu, s5_b_glu)
    y_b = window_branch(q_b, k_b, v_b, win_qn, win_kn, win_sink, cos, sin)
    y_c = diff_branch(q_c, k_c, v_c, diff_qn, diff_kn, diff_lambda, diff_subln, lam_init, cos, sin)
    y_d = neighborhood_branch(q_d, k_d, v_d, na_qn, na_kn, na_rpb)
    gate_parts = jnp.split(gates, N_BRANCH, axis=-1)
    merged = None
    for i, y in enumerate((y_a, y_b, y_c, y_d)):
        term = jax.nn.sigmoid(gate_parts[i]) * (y @ w_branch[i])
        merged = term if merged is None else merged + term
    return merged @ w_out


def hier_moe(h, w_group, b_group, w_expert, b_expert, w1, w3, w2):
    bsz, tlen, dm = h.shape
    hf = h.reshape(bsz * tlen, dm)
    g_prob = softmax_f32(hf @ w_group + b_group)
    g_w, g_idx = lax.top_k(g_prob, 1)
    e_logits = (hf @ w_expert + b_expert).astype(jnp.float32).reshape(-1, N_GROUPS, EXPERTS_PER_GROUP)
    e_in = jnp.take_along_axis(e_logits, g_idx[:, :, None], axis=1)[:, 0]
    top_v, top_i = lax.top_k(e_in, TOP_K_IN_GROUP)
    w_sel = jax.nn.softmax(top_v, axis=-1) * g_w
    expert_id = g_idx * EXPERTS_PER_GROUP + top_i
    combine = jnp.sum(jax.nn.one_hot(expert_id, N_EXPERTS, dtype=jnp.float32) * w_sel[..., None], axis=1).astype(h.dtype)
    out = None
    for e in range(N_EXPERTS):
        ye = (jax.nn.silu(hf @ w1[e]) * (hf @ w3[e])) @ w2[e]
        term = combine[:, e:e + 1] * ye
        out = term if out is None else out + term
    return out.reshape(bsz, tlen, dm)


def modulated_norm(xc, xl, gain, shift_c, scale_c, shift_l, scale_l):
    hc = rms_norm(xc, gain) * (1.0 + scale_c) + shift_c
    hl = rms_norm(xl, gain) * (1.0 + scale_l[:, None]) + shift_l[:, None]
    return jnp.concatenate([hc, hl], axis=1)


def setup_inputs(seed: int = 0) -> dict:
    key = jax.random.key(seed)
    keys = jax.random.split(key, 38)
    f32 = jnp.float32
    D = D_MODEL

    def nrm(i, shape, scale):
        return scale * jax.random.normal(keys[i], shape, f32)

    n_idx = jnp.arange(S5_STATE, dtype=f32)
    s5_shape = (DEPTH, 2, S5_GROUPS, S5_STATE)
    return {
        'x': nrm(0, (BATCH, SEQ, D), 1.0),
        'c': nrm(1, (BATCH, D), 1.0),
        'ctx': nrm(2, (BATCH, CTX_LEN, D), 1.0),
        'c_ctx': nrm(3, (D,), 1.0),
        'w_ada': nrm(4, (DEPTH, D, 6 * D), 0.5 * D ** -0.5),
        'b_ada': nrm(5, (DEPTH, 6 * D), 0.02),
        'norm_mix': 1.0 + nrm(6, (DEPTH, D), 0.02),
        'norm_ffn': 1.0 + nrm(7, (DEPTH, D), 0.02),
        'w_in': nrm(8, (DEPTH, D, IN_WIDTH), D ** -0.5),
        's5_a_re': -0.5 + nrm(9, s5_shape, 0.01),
        's5_a_im': math.pi * n_idx + nrm(10, s5_shape, 0.01),
        's5_log_step': jax.random.uniform(keys[11], (DEPTH, 2, S5_GROUPS), f32, math.log(1e-3), math.log(1e-1)),
        's5_b_re': nrm(12, (DEPTH, 2, S5_GROUPS, S5_STATE, S5_CH), (2 * S5_CH) ** -0.5),
        's5_b_im': nrm(13, (DEPTH, 2, S5_GROUPS, S5_STATE, S5_CH), (2 * S5_CH) ** -0.5),
        's5_c_re': nrm(14, (DEPTH, 2, S5_GROUPS, S5_CH, S5_STATE), S5_STATE ** -0.5),
        's5_c_im': nrm(15, (DEPTH, 2, S5_GROUPS, S5_CH, S5_STATE), S5_STATE ** -0.5),
        's5_d': nrm(16, (DEPTH, BRANCH_W), 0.5),
        's5_w_glu': nrm(17, (DEPTH, BRANCH_W, BRANCH_W), BRANCH_W ** -0.5),
        's5_b_glu': nrm(18, (DEPTH, BRANCH_W), 0.02),
        'win_qn': 1.0 + nrm(19, (DEPTH, HEAD_DIM), 0.02),
        'win_kn': 1.0 + nrm(20, (DEPTH, HEAD_DIM), 0.02),
        'win_sink': nrm(21, (DEPTH, WIN_HEADS), 0.5),
        'diff_qn': 1.0 + nrm(22, (DEPTH, HEAD_DIM), 0.02),
        'diff_kn': 1.0 + nrm(23, (DEPTH, HEAD_DIM), 0.02),
        'diff_lambda': nrm(24, (DEPTH, 4, HEAD_DIM), 0.1),
        'diff_subln': 1.0 + nrm(25, (DEPTH, DIFF_VDIM), 0.02),
        'na_qn': 1.0 + nrm(26, (DEPTH, HEAD_DIM), 0.02),
        'na_kn': 1.0 + nrm(27, (DEPTH, HEAD_DIM), 0.02),
        'na_rpb': nrm(28, (DEPTH, NA_HEADS, 2 * NA_WIN_R - 1, 2 * NA_WIN_C - 1), 0.1),
        'w_branch': nrm(29, (DEPTH, N_BRANCH, BRANCH_W, D), BRANCH_W ** -0.5),
        'w_out': nrm(30, (DEPTH, D, D), D ** -0.5),
        'moe_w_group': nrm(31, (DEPTH, D, N_GROUPS), D ** -0.5),
        'moe_b_group': nrm(32, (DEPTH, N_GROUPS), 0.01),
        'moe_w_expert': nrm(33, (DEPTH, D, N_EXPERTS), D ** -0.5),
        'moe_b_expert': nrm(34, (DEPTH, N_EXPERTS), 0.01),
        'moe_w1': nrm(35, (DEPTH, N_EXPERTS, D, EXPERT_FF), D ** -0.5),
        'moe_w3': nrm(36, (DEPTH, N_EXPERTS, D, EXPERT_FF), D ** -0.5),
        'moe_w2': nrm(37, (DEPTH, N_EXPERTS, EXPERT_FF, D), EXPERT_FF ** -0.5),
    }


def reference(x, c, ctx, c_ctx, w_ada, b_ada, norm_mix, norm_ffn, w_in,
              s5_a_re, s5_a_im, s5_log_step, s5_b_re, s5_b_im, s5_c_re, s5_c_im, s5_d, s5_w_glu, s5_b_glu,
              win_qn, win_kn, win_sink, diff_qn, diff_kn, diff_lambda, diff_subln,
              na_qn, na_kn, na_rpb, w_branch, w_out,
              moe_w_group, moe_b_group, moe_w_expert, moe_b_expert, moe_w1, moe_w3, moe_w2):
    n_lat = x.shape[1]
    cos, sin = axial_rope_tables(n_lat)
    xc, xl = ctx, x
    for layer in range(DEPTH):
        lam_init = 0.8 - 0.6 * math.exp(-0.3 * layer)
        mod_l = jnp.split(jax.nn.silu(c) @ w_ada[layer] + b_ada[layer], 6, axis=-1)
        mod_c = jnp.split(jax.nn.silu(c_ctx) @ w_ada[layer] + b_ada[layer], 6, axis=-1)
        h = modulated_norm(xc, xl, norm_mix[layer], mod_c[0], mod_c[1], mod_l[0], mod_l[1])
        mix = hybrid_mixer(h, w_in[layer], w_branch[layer], w_out[layer],
                           s5_a_re[layer], s5_a_im[layer], s5_log_step[layer], s5_b_re[layer], s5_b_im[layer],
                           s5_c_re[layer], s5_c_im[layer], s5_d[layer], s5_w_glu[layer], s5_b_glu[layer],
                           win_qn[layer], win_kn[layer], win_sink[layer],
                           diff_qn[layer], diff_kn[layer], diff_lambda[layer], diff_subln[layer], lam_init,
                           na_qn[layer], na_kn[layer], na_rpb[layer], cos, sin)
        xc = xc + mod_c[2] * mix[:, :CTX_LEN]
        xl = xl + mod_l[2][:, None] * mix[:, CTX_LEN:]
        h = modulated_norm(xc, xl, norm_ffn[layer], mod_c[3], mod_c[4], mod_l[3], mod_l[4])
        ff = hier_moe(h, moe_w_group[layer], moe_b_group[layer], moe_w_expert[layer], moe_b_expert[layer],
                      moe_w1[layer], moe_w3[layer], moe_w2[layer])
        xc = xc + mod_c[5] * ff[:, :CTX_LEN]
        xl = xl + mod_l[5][:, None] * ff[:, CTX_LEN:]
    return xl
```

```python
import contextlib
import math
import numpy as np
import ml_dtypes
import concourse.bass as bass
import concourse.mybir as mybir
from concourse.bass_utils import run_bass_kernel_spmd

F32 = mybir.dt.float32
BF16 = mybir.dt.bfloat16
ALU = mybir.AluOpType
AF = mybir.ActivationFunctionType
AX = mybir.AxisListType

D = 2048
T = 2304
NT = 18
NCT = 2
DEPTH = 4
INW = 12544
NPROJ = 4352
EPS = 1e-6
NEG = -30000.0

ENGS = ("pe", "act", "dve", "pool", "sp")
SEM_ROT = 30000


class Prog:
    def __init__(self, nc):
        self.nc = nc
        self.ops = {e: [] for e in ENGS}
        self.lastw = {}
        self.readers = {}
        self.dsem = {}
        self.pending_fence = {e: set() for e in ENGS}

    def _deps(self, r, w):
        deps = set()
        for k in r:
            if k in self.lastw:
                deps.add(self.lastw[k])
        for k in w:
            if k in self.lastw:
                deps.add(self.lastw[k])
            for x in self.readers.get(k, ()):
                deps.add(x)
        return deps

    def _commit(self, me, r, w):
        for k in w:
            self.lastw[k] = me
            self.readers[k] = []
        for k in r:
            if k in w:
                continue
            self.readers.setdefault(k, []).append(me)

    def fence(self):
        deps = set()
        for e in ENGS:
            for i in range(len(self.ops[e]) - 1, -1, -1):
                if self.ops[e][i]["kind"] == "c":
                    deps.add(("e", e, i))
                    break
        for s, c in self.dsem.items():
            deps.add(("d", s, c))
        for e in ENGS:
            self.pending_fence[e] |= deps

    def op(self, eng, fn, r=(), w=(), nosame=False):
        r = tuple(r); w = tuple(w)
        deps = self._deps(r, w)
        if eng == "pe" or nosame:
            deps = {d for d in deps if not (d[0] == "e" and d[1] == eng)}
        deps |= self.pending_fence[eng]
        self.pending_fence[eng] = set()
        idx = len(self.ops[eng])
        me = ("e", eng, idx)
        deps.discard(me)
        self.ops[eng].append(dict(kind="c", fn=fn, deps=deps, sig=False))
        self._commit(me, r, w)
        return me

    def dma(self, q, fns, sem, r=(), w=()):
        r = tuple(r); w = tuple(w)
        deps = self._deps(r, w)
        deps |= self.pending_fence[q]
        self.pending_fence[q] = set()
        cnt = self.dsem.get(sem, 0) + 16 * len(fns)
        self.dsem[sem] = cnt
        me = ("d", sem, cnt)
        self.ops[q].append(dict(kind="d", fns=fns, deps=deps, sem=sem))
        self._commit(me, r, w)
        return me

    def emit(self, final_sems=()):
        nc = self.nc
        for e in ENGS:
            for o in self.ops[e]:
                for d in o["deps"]:
                    if d[0] == "e":
                        self.ops[d[1]][d[2]]["sig"] = True
        signo = {}
        nsig = {}
        for e in ENGS:
            n = 0
            for i, o in enumerate(self.ops[e]):
                if o["kind"] == "c" and o["sig"]:
                    n += 1
                    signo[(e, i)] = n
            nsig[e] = n
        with contextlib.ExitStack() as st:
            esems = {}
            for e in ENGS:
                k = max(1, (nsig[e] + SEM_ROT - 1) // SEM_ROT)
                esems[e] = [st.enter_context(nc.semaphore(f"s_{e}{j}")) for j in range(k)]
            dsems = {name: st.enter_context(nc.semaphore(f"d_{name}")) for name in self.dsem}
            self.nsems = sum(len(v) for v in esems.values()) + len(dsems)

            def semval(d):
                if d[0] == "e":
                    n = signo[(d[1], d[2])]
                    j = (n - 1) // SEM_ROT
                    return (("e", d[1], j), esems[d[1]][j], n - j * SEM_ROT)
                return (("d", d[1]), dsems[d[1]], d[2])

            block = st.enter_context(nc.Block())

            def run_queue(e, eng):
                seen = {}
                for i, o in enumerate(self.ops[e]):
                    need = {}
                    for d in o["deps"]:
                        key, sem, val = semval(d)
                        if seen.get(key, 0) >= val:
                            continue
                        if need.get(key, (None, 0))[1] < val:
                            need[key] = (sem, val)
                    for key, (sem, val) in need.items():
                        eng.wait_ge(sem, val)
                        seen[key] = val
                    if o["kind"] == "c":
                        ins = o["fn"](eng)
                        if o["sig"]:
                            n = signo[(e, i)]
                            ins.then_inc(esems[e][(n - 1) // SEM_ROT], 1)
                    else:
                        for f in o["fns"]:
                            f(eng).then_inc(dsems[o["sem"]], 16)
                if e == "sp":
                    for name in final_sems:
                        eng.wait_ge(dsems[name], self.dsem[name])

            @block.tensor
            def _(eng):
                run_queue("pe", eng)

            @block.scalar
            def _(eng):
                run_queue("act", eng)

            @block.vector
            def _(eng):
                run_queue("dve", eng)

            @block.gpsimd
            def _(eng):
                run_queue("pool", eng)

            @block.sync
            def _(eng):
                run_queue("sp", eng)


class SB:
    def __init__(self, nc, base=16512, limit=229312):
        self.nc = nc
        self.off = base
        self.limit = limit
        self.n = 0
        self.prog = None
        self.dirty = False

    def alloc(self, name, shape, dtype):
        if self.dirty:
            self.prog.fence()
            self.dirty = False
        sz = 1
        for s in shape[1:]:
            sz *= s
        nbytes = sz * (2 if dtype == BF16 else 4)
        nbytes = (nbytes + 63) // 64 * 64
        off = self.off
        assert off + nbytes <= self.limit, f"SBUF overflow at {name}: {off}+{nbytes}"
        self.off += nbytes
        self.n += 1
        return self.nc.alloc_sbuf_tensor_at(f"{name}_{self.n}", list(shape), dtype, offset=off)

    def mark(self):
        return self.off

    def release(self, m):
        self.off = m
        self.dirty = True


class Ctx:
    pass


def _mk(C):
    p = C.p

    def mm(out, lhsT, rhs, start, stop, r, w):
        p.op("pe", lambda e: e.matmul(out, lhsT=lhsT, rhs=rhs, start=start, stop=stop), r, w)

    C.bank_open = {}

    def amm(bank, out, lhsT, rhs, r, w):
        first = not C.bank_open.get(bank, False)
        C.bank_open[bank] = True
        p.op("pe", lambda e: e.matmul(out, lhsT=lhsT, rhs=rhs, start=first, stop=False, skip_group_check=True), r, w)

    def adone(bank):
        C.bank_open[bank] = False

    C.amm, C.adone = amm, adone

    def tr(out, in_, ident, r, w):
        p.op("pe", lambda e: e.transpose(out=out, in_=in_, identity=ident), r, w)

    def act(out, in_, func, r, w, scale=1.0, bias=None, accum=None, nosame=False):
        def f(e):
            kw = {}
            if bias is not None:
                kw["bias"] = bias
            if accum is not None:
                kw["accum_out"] = accum
            return e.activation(out=out, in_=in_, func=func, scale=scale, **kw)
        p.op("act", f, r, w, nosame=nosame)

    def tt(eng, out, a, b, op, r, w):
        p.op(eng, lambda e: e.tensor_tensor(out=out, in0=a, in1=b, op=op), r, w)

    def ts(eng, out, a, s1, s2, op0, op1, r, w):
        if op1 is None:
            p.op(eng, lambda e: e.tensor_scalar(out=out, in0=a, scalar1=s1, scalar2=None, op0=op0), r, w)
        else:
            p.op(eng, lambda e: e.tensor_scalar(out=out, in0=a, scalar1=s1, scalar2=s2, op0=op0, op1=op1), r, w)

    def stt(out, a, s, b, op0, op1, r, w):
        p.op("dve", lambda e: e.scalar_tensor_tensor(out=out, in0=a, scalar=s, in1=b, op0=op0, op1=op1), r, w)

    def cp(eng, out, in_, r, w):
        if eng == "act":
            p.op("act", lambda e: e.activation(out=out, in_=in_, func=AF.Copy), r, w)
        else:
            p.op(eng, lambda e: e.tensor_copy(out=out, in_=in_), r, w)

    def red(out, in_, op, r, w, axis=AX.X):
        p.op("dve", lambda e: e.tensor_reduce(out=out, in_=in_, axis=axis, op=op), r, w)

    def recip(out, in_, r, w):
        p.op("dve", lambda e: e.reciprocal(out=out, in_=in_), r, w)

    def memset(eng, ap, val, w):
        p.op(eng, lambda e: e.memset(ap, val), (), w)

    def dma(q, out, in_, sem, r, w):
        p.dma(q, [lambda e: e.dma_start(out=out, in_=in_)], sem, r, w)

    C.mm, C.tr, C.act, C.tt, C.ts, C.stt, C.cp, C.red, C.recip, C.memset, C.dma = \
        mm, tr, act, tt, ts, stt, cp, red, recip, memset, dma


def tok_lc(tt):
    return 1 if tt < NCT else 0


def build_program(n_layers=DEPTH, stop_after=None, dbg=(), only=None, skip_front=False):
    nc = bass.Bass("TRN2", target_bir_lowering=False)
    C = Ctx()
    C.nc = nc
    C.p = Prog(nc)
    _mk(C)
    p = C.p

    def dram_in(name, shape, dtype=F32):
        return nc.dram_tensor(name, list(shape), dtype, kind="ExternalInput").ap()

    def dram_tmp(name, shape, dtype=F32):
        kind = "ExternalOutput" if name in dbg else "Internal"
        return nc.dram_tensor(name, list(shape), dtype, kind=kind).ap()

    I = {}
    C.I = I
    I["x"] = dram_in("x", [2048, D])
    I["ctx"] = dram_in("ctx", [256, D])
    I["cT"] = dram_in("cT", [128, 16, 2])
    I["w_ada"] = dram_in("w_ada", [n_layers, D, 6 * D])
    I["b_adaT"] = dram_in("b_adaT", [n_layers, 128, 96])
    I["normT"] = dram_in("normT", [n_layers, 2, 128, 16])
    I["w_in"] = dram_in("w_in", [n_layers, D, INW])
    I["rope"] = dram_in("rope", [128, 16, 2, 32])
    I["wmask"] = dram_in("wmask", [2, 128, 512])
    I["qkg"] = dram_in("qkg", [n_layers, 6 * 64])
    I["sink"] = dram_in("sink", [n_layers, 8])
    I["dlam"] = dram_in("dlam", [n_layers, 4 * 64])
    I["subg"] = dram_in("subg", [n_layers, 128])
    I["nab"] = dram_in("nab", [n_layers, 5, 128, 8 * 5 * 128])
    I["s5a"] = dram_in("s5a", [n_layers, 128, 32, 2])
    I["s5ls"] = dram_in("s5ls", [n_layers, 128, 32])
    I["s5b"] = dram_in("s5b", [n_layers, 128, 32, 2, 16])
    I["s5c"] = dram_in("s5c", [n_layers, 128, 32, 2, 16])
    I["s5d"] = dram_in("s5d", [n_layers, 128, 32])
    I["s5mask"] = dram_in("s5mask", [2, 128, 128])
    I["s5_w_glu"] = dram_in("s5_w_glu", [n_layers, 512, 512])
    I["s5_b_glu"] = dram_in("s5_b_glu", [n_layers, 512])
    I["w_branch"] = dram_in("w_branch", [n_layers, 2048, 2048])
    I["w_out"] = dram_in("w_out", [n_layers, 2048, 2048])
    I["wrT"] = dram_in("wrT", [n_layers, 128, 16 * 20])
    I["brt"] = dram_in("brt", [n_layers, 20])
    I["moe_w1"] = dram_in("moe_w1", [n_layers, 16, 2048, 1024])
    I["moe_w3"] = dram_in("moe_w3", [n_layers, 16, 2048, 1024])
    I["moe_w2"] = dram_in("moe_w2", [n_layers, 16, 1024, 2048])
    out = nc.dram_tensor("out", [2048, D], F32, kind="ExternalOutput").ap()

    XR = dram_tmp("XR", [T, D])
    PROJ = dram_tmp("PROJ", [T, NPROJ])
    SG = dram_tmp("SG", [64, 128, T], BF16)
    MODD = dram_tmp("MODD", [128, 96, 2])
    YT = dram_tmp("YT", [16, 128, T], BF16)
    YS = dram_tmp("YS", [T, 512])
    MG = dram_tmp("MG", [16, 128, T], BF16)
    HT2 = dram_tmp("HT2", [128, 16, T], BF16)
    CW = dram_tmp("CW", [128, NT, 16])

    sb = SB(nc)
    sb.prog = p
    PS = [nc.alloc_psum_tensor(f"ps{i}", [128, 512], F32) for i in range(8)]
    C.PS = PS
    ident = sb.alloc("ident", [128, 128], F32)
    identb = sb.alloc("identb", [128, 128], BF16)
    C.memset("pool", ident[:, :], 0.0, ["ident"])
    p.op("pool", lambda e: e.affine_select(out=ident[:, :], in_=ident[:, :], pattern=[[-1, 128]], compare_op=ALU.not_equal,
                                           fill=1.0, base=0, channel_multiplier=1), ["ident"], ["ident"])
    C.cp("pool", identb[:, :], ident[:, :], ["ident"], ["identb"])
    ones = sb.alloc("ones", [128, 128], F32)
    C.memset("pool", ones[:, :], 1.0, ["ones"])
    scT = sb.alloc("scT", [128, 16, 2], F32)
    C.dma("sp", scT[:, :, :], I["cT"][:, :, :], "scT", [], ["scT"])
    C.act(scT[:, :, :], scT[:, :, :], AF.Silu, ["scT"], ["scT"])
    modT = sb.alloc("modT", [128, 96, 2], F32)
    Amod = sb.alloc("Amod", [128, 2, 16, 2], F32)
    normg = sb.alloc("normg", [128, 2, 16], F32)
    bada = sb.alloc("bada", [128, 96], F32)
    gmark = sb.mark()

    m_init = sb.mark()
    cpb = [sb.alloc(f"cpb{i}", [128, D], F32) for i in range(2)]
    for tt in range(NT):
        i = tt % 2
        src = I["ctx"][tt * 128:(tt + 1) * 128, :] if tt < NCT else I["x"][(tt - NCT) * 128:(tt - NCT + 1) * 128, :]
        C.dma("sp", cpb[i][:, :], src, f"cpb{i}", [], [f"cpb{i}"])
        C.dma("sp", XR[tt * 128:(tt + 1) * 128, :], cpb[i][:, :], f"cpo{i}", [f"cpb{i}"], [("XR", tt)])
    sb.release(m_init)
    p.fence()

    def phase_mod(l):
        m0 = sb.mark()
        wb = [sb.alloc(f"wada{i}", [128, 16, 256], F32) for i in range(2)]
        C.dma("sp", bada[:, :], I["b_adaT"][l, :, :], "bada", [], ["bada"])
        C.dma("sp", normg[:, :, :], I["normT"][l].rearrange("s p k -> p s k"), "normg", [], ["normg"])
        pm = PS[7][:, 0:192].rearrange("p (m c) -> p m c", c=2)
        wsrc = I["w_ada"][l].rearrange("(kc p) n -> p kc n", p=128)
        for blk in range(48):
            b = wb[blk % 2]
            C.dma("sp", b[:, :, :], wsrc[:, :, blk * 256:(blk + 1) * 256], f"wada{blk % 2}", [], [f"wada{blk % 2}"])
            for mi in range(2):
                m = blk * 2 + mi
                for kc in range(16):
                    C.mm(pm[:, m, :], b[:, kc, mi * 128:(mi + 1) * 128], scT[:, kc, :], kc == 0, kc == 15,
                         [f"wada{blk % 2}", "scT"], ["ps7"])
        C.tt("dve", modT[:, :, :], pm, bada[:, :].unsqueeze(2).to_broadcast([128, 96, 2]), ALU.add, ["ps7", "bada"], ["modT"])
        for s in range(2):
            j = 1 + 3 * s
            C.ts("dve", Amod[:, s, :, :], modT[:, j * 16:(j + 1) * 16, :], 1.0, None, ALU.add, None, ["modT"], ["Amod"])
            C.tt("dve", Amod[:, s, :, :], Amod[:, s, :, :], normg[:, s, :].unsqueeze(2).to_broadcast([128, 16, 2]), ALU.mult,
                 ["Amod", "normg"], ["Amod"])
        if "MODD" in dbg:
            C.dma("sp", MODD[:, :, :], modT[:, :, :], "dbg", ["modT"], ["MODD"])
        sb.release(m0)

    def phase_norm(l, s, hT, router=None):
        m0 = sb.mark()
        xt = [sb.alloc(f"xt{i}", [128, D], F32) for i in range(2)]
        junk = sb.alloc("junk", [128, D], BF16)
        st = [sb.alloc(f"nst{i}", [128, 4], F32) for i in range(2)]
        import os
        for tt in (range(NT) if not os.environ.get("REVN") else reversed(range(NT))):
            i = tt % 2
            lc = tok_lc(tt)
            x_, s_ = xt[i], st[i]
            C.dma("sp", x_[:, :], XR[tt * 128:(tt + 1) * 128, :], f"xt{i}", [("XR", tt)], [f"xt{i}"])
            C.act(junk[:, :], x_[:, :], AF.Square, [f"xt{i}"], ["junk", f"nst{i}a"], accum=s_[:, 0:1])
            C.ts("dve", s_[:, 1:2], s_[:, 0:1], 1.0 / D, EPS, ALU.mult, ALU.add, [f"nst{i}a"], [f"nst{i}b"])
            C.act(s_[:, 2:3], s_[:, 1:2], AF.Sqrt, [f"nst{i}b"], [f"nst{i}c"])
            C.recip(s_[:, 3:4], s_[:, 2:3], [f"nst{i}c"], [f"nst{i}d"])
            C.ts("dve", x_[:, :], x_[:, :], s_[:, 3:4], None, ALU.mult, None, [f"xt{i}", f"nst{i}d"], [f"xt{i}"])
            for g4 in range(4):
                pb = 2 + (g4 % 2)
                pT = PS[pb][:, :].rearrange("p (a b) -> p a b", b=128)
                for a in range(4):
                    kc = g4 * 4 + a
                    C.tr(pT[:, a, :], x_[:, kc * 128:(kc + 1) * 128], ident[:, :], [f"xt{i}", "ident"], [f"ps{pb}"])
                for a in range(4):
                    kc = g4 * 4 + a
                    C.act(hT[:, kc, tt * 128:(tt + 1) * 128], pT[:, a, :], AF.Identity, [f"ps{pb}", "Amod", "modT"],
                          [("hT", tt)], scale=Amod[:, s, kc, lc:lc + 1], bias=modT[:, (3 * s) * 16 + kc, lc:lc + 1], nosame=True)
        sb.release(m0)

    def phase_inproj(l, hT):
        m0 = sb.mark()
        wb = [sb.alloc(f"win{i}", [128, 16, 512], BF16) for i in range(2)]
        ev = [sb.alloc(f"pev{i}", [128, 512], F32) for i in range(2)]
        sg = [sb.alloc(f"sgv{i}", [128, 512], BF16) for i in range(2)]
        wsrc = I["w_in"][l].rearrange("(kc p) n -> p kc n", p=128)
        nblk = 0
        cnt = 0
        for c0 in range(0, NPROJ, 512):
            cw = min(512, NPROJ - c0)
            b = wb[nblk % 2]; bk = f"win{nblk % 2}"
            C.dma("pool", b[:, :, 0:cw], wsrc[:, :, c0:c0 + cw], bk, [], [bk])
            nblk += 1
            for tt in range(NT):
                pb = cnt % 2
                for kc in range(16):
                    C.mm(PS[pb][:, 0:cw], hT[:, kc, tt * 128:(tt + 1) * 128], b[:, kc, 0:cw], kc == 0, kc == 15,
                         [("hT", tt), bk], [f"ps{pb}"])
                e_ = ev[cnt % 2]; ek = f"pev{cnt % 2}"
                C.cp("dve" if cnt % 2 == 0 else "act", e_[:, 0:cw], PS[pb][:, 0:cw], [f"ps{pb}"], [ek])
                C.dma("sp", PROJ[tt * 128:(tt + 1) * 128, c0:c0 + cw], e_[:, 0:cw], ek, [ek], [("PROJ", tt, c0 // 512)])
                cnt += 1
        tchunks = [(0, 512), (512, 512), (1024, 512), (1536, 512), (2048, 256)]
        for g4 in range(16):
            c0 = NPROJ + g4 * 512
            b = wb[nblk % 2]; bk = f"win{nblk % 2}"
            C.dma("pool", b[:, :, :], wsrc[:, :, c0:c0 + 512], bk, [], [bk])
            nblk += 1
            for a in range(4):
                gi = g4 * 4 + a
                for (t0, tw) in tchunks:
                    pb = cnt % 2
                    for kc in range(16):
                        C.mm(PS[pb][:, 0:tw], b[:, kc, a * 128:(a + 1) * 128], hT[:, kc, t0:t0 + tw], kc == 0, kc == 15,
                             [("hT", t_) for t_ in range(NT)] + [bk], [f"ps{pb}"])
                    s_ = sg[cnt % 2]; sk = f"sgv{cnt % 2}"
                    C.act(s_[:, 0:tw], PS[pb][:, 0:tw], AF.Sigmoid, [f"ps{pb}"], [sk])
                    C.dma("sp", SG[gi, :, t0:t0 + tw], s_[:, 0:tw], sk, [sk], [("SG", gi)])
                    cnt += 1
        sb.release(m0)


    def bcast_load(dst, src_row, n, sem, key):
        C.dma("sp", dst, src_row.partition_broadcast(128), sem, [], [key])

    def prep_qk(l, col0, nh, gidx, rope, dst, qscale=None, dup=False, tag="q"):
        m0 = sb.mark()
        W = nh * 64
        src = [sb.alloc(f"pq_src{i}", [128, W], F32) for i in range(2)]
        tmp = [sb.alloc(f"pq_tmp{i}", [128, W], F32) for i in range(2)]
        o = [sb.alloc(f"pq_o{i}", [128, (2 * W if dup else W)], F32) for i in range(2)]
        stt_ = [sb.alloc(f"pq_st{i}", [128, 4, 8], F32) for i in range(2)]
        gB = sb.alloc("pq_g", [128, 64], F32)
        bcast_load(gB[:, :], I["qkg"][l, gidx * 64:(gidx + 1) * 64], 64, "pq_g", "pq_g")
        if qscale is not None:
            C.ts("dve", gB[:, :], gB[:, :], qscale, None, ALU.mult, None, ["pq_g"], ["pq_g"])
        cs = sb.alloc("pq_cs", [128, 16, 2, 32], F32)
        if rope:
            C.dma("sp", cs[:, :, :, :], I["rope"][:, :, :, :], "pq_cs", [], ["pq_cs"])
        for tt in range(NT):
            i = tt % 2
            S_, T_, O_, st_ = src[i], tmp[i], o[i], stt_[i]
            ks, kt_, ko, kst = f"pq_src{i}", f"pq_tmp{i}", f"pq_o{i}", f"pq_st{i}"
            C.dma("sp", S_[:, :], PROJ[tt * 128:(tt + 1) * 128, col0:col0 + W], ks, [("PROJ", tt, c) for c in range(9)], [ks])
            S3 = S_[:, :].rearrange("p (h d) -> p h d", d=64)
            T3 = T_[:, :].rearrange("p (h d) -> p h d", d=64)
            C.tt("dve", T_[:, :], S_[:, :], S_[:, :], ALU.mult, [ks], [kt_])
            C.red(st_[:, 0, 0:nh], T3, ALU.add, [kt_], [kst])
            C.ts("dve", st_[:, 1, 0:nh], st_[:, 0, 0:nh], 1.0 / 64, EPS, ALU.mult, ALU.add, [kst], [kst])
            C.act(st_[:, 2, 0:nh], st_[:, 1, 0:nh], AF.Sqrt, [kst], [kst])
            C.recip(st_[:, 3, 0:nh], st_[:, 2, 0:nh], [kst], [kst])
            C.tt("dve", T3, S3, st_[:, 3, 0:nh].unsqueeze(2).to_broadcast([128, nh, 64]), ALU.mult, [ks, kst], [kt_])
            C.tt("dve", T3, T3, gB[:, :].unsqueeze(1).to_broadcast([128, nh, 64]), ALU.mult, [kt_, "pq_g"], [kt_])
            if dup:
                O4 = O_[:, :].rearrange("p (h u d) -> p h u d", u=2, d=64)
                Of = [O4[:, :, 0, :], O4[:, :, 1, :]]
            else:
                Of = [O_[:, :].rearrange("p (h d) -> p h d", d=64)]
            if rope and tt >= NCT:
                n = tt - NCT
                cosb = cs[:, n, 0, :].unsqueeze(1).to_broadcast([128, nh, 32])
                sinb = cs[:, n, 1, :].unsqueeze(1).to_broadcast([128, nh, 32])
                x1 = T3[:, :, 0:32]; x2 = T3[:, :, 32:64]
                A3 = S3
                C.tt("dve", A3[:, :, 0:32], x1, cosb, ALU.mult, [kt_, "pq_cs"], [ks])
                C.tt("dve", A3[:, :, 32:64], x2, sinb, ALU.mult, [kt_, "pq_cs"], [ks])
                for Oo in Of:
                    C.tt("dve", Oo[:, :, 0:32], A3[:, :, 0:32], A3[:, :, 32:64], ALU.subtract, [ks], [ko])
                C.tt("dve", A3[:, :, 0:32], x1, sinb, ALU.mult, [kt_, "pq_cs", ko], [ks])
                C.tt("dve", A3[:, :, 32:64], x2, cosb, ALU.mult, [kt_, "pq_cs"], [ks])
                for Oo in Of:
                    C.tt("dve", Oo[:, :, 32:64], A3[:, :, 0:32], A3[:, :, 32:64], ALU.add, [ks], [ko])
            else:
                for Oo in Of:
                    C.cp("dve", Oo, T3, [kt_], [ko])
            npair = (2 * W if dup else W) // 128
            for g4 in range(0, npair, 4):
                pb = 2 + ((g4 // 4) % 2)
                na = min(4, npair - g4)
                pT = PS[pb][:, :].rearrange("p (a b) -> p a b", b=128)
                for a in range(na):
                    C.tr(pT[:, a, :], O_[:, (g4 + a) * 128:(g4 + a + 1) * 128], ident[:, :], [ko, "ident"], [f"ps{pb}"])
                C.act(dst[:, g4:g4 + na, tt * 128:(tt + 1) * 128], pT[:, 0:na, :], AF.Copy, [f"ps{pb}"], [(tag, tt)])
        sb.release(m0)

    def load_v(col0, nkv, dv, Vt, key):
        C.memset("dve", Vt[:, :, :, dv:dv + 1], 1.0, [key])
        for t in range(NT):
            p.dma("pool", [lambda e, t=t: e.dma_start(
                out=Vt[:, t, :, 0:dv],
                in_=PROJ[t * 128:(t + 1) * 128, col0:col0 + nkv * dv].rearrange("p (h d) -> p h d", d=dv))],
                key, [("PROJ", t, c) for c in range(9)] + [key], [key])

    def emit_y(ytile, branch, tq, ykey):
        yb = C.yb[tq % 2]; ybk = f"yb{tq % 2}"
        C.cp("dve", yb[:, :], ytile, [ykey], [ybk])
        pTb = PS[6][:, :].bitcast(BF16).rearrange("p (a b) -> p a b", b=128)
        for a in range(4):
            C.tr(pTb[:, a, :], yb[:, a * 128:(a + 1) * 128], identb[:, :], [ybk, "identb"], ["ps6"])
        yt = C.ytb[tq % 2]; ytk = f"ytb{tq % 2}"
        C.cp("act", yt[:, :, :], pTb[:, 0:4, :], ["ps6"], [ytk])
        C.dma("sp", YT[branch * 4:(branch + 1) * 4, :, tq * 128:(tq + 1) * 128].rearrange("k p t -> p k t"), yt[:, :, :],
              ytk, [ytk], [("YT", branch, tq)])

    def alloc_y():
        C.yb = [sb.alloc(f"yb{i}", [128, 512], BF16) for i in range(2)]
        C.ytb = [sb.alloc(f"ytb{i}", [128, 4, 128], BF16) for i in range(2)]

    def phase_win(l):
        m0 = sb.mark()
        qT = sb.alloc("w_qT", [128, 4, T], BF16)
        kT = sb.alloc("w_kT", [128, 2, T], BF16)
        Vt = sb.alloc("w_V", [128, NT, 2, 65], BF16)
        mk = sb.alloc("w_mask", [128, 2, 512], BF16)
        sk = sb.alloc("w_sink", [128, 8], F32)
        p.dma("pool", [lambda e: e.dma_start(out=mk[:, :, :], in_=I["wmask"].rearrange("m p c -> p m c"))], "w_mask", [], ["w_mask"])
        bcast_load(sk[:, :], I["sink"][l, :], 8, "w_sink", "w_sink")
        C.act(sk[:, :], sk[:, :], AF.Exp, ["w_sink"], ["w_sink"])
        load_v(1152, 2, 64, Vt, "w_V")
        prep_qk(l, 512, 8, 0, True, qT, tag="w_q")
        prep_qk(l, 1024, 2, 1, True, kT, dup=True, tag="w_k")
        alloc_y()
        E = [sb.alloc(f"w_E{i}", [128, 512], BF16) for i in range(2)]
        ysb = [sb.alloc(f"w_y{i}", [128, 512], F32) for i in range(2)]
        den = [sb.alloc(f"w_den{i}", [128, 8], F32) for i in range(2)]
        allq = [("w_q", t) for t in range(NT)]; allk = [("w_k", t) for t in range(NT)]
        ecnt = 0
        for tq in range(NT):
            if tq < NCT:
                keys = [(0, None), (1, None)]
            else:
                n = tq - NCT
                keys = [(0, None), (1, None)]
                if n >= 1:
                    keys.append((tq - 1, 0))
                keys.append((tq, None))
                if n <= 14:
                    keys.append((tq + 1, 1))
            y_ = ysb[tq % 2]; yk = f"w_y{tq % 2}"
            d_ = den[tq % 2]; dk = f"w_den{tq % 2}"
            for kh in range(2):
                pO = PS[4 + kh][:, 0:260].rearrange("p (j c) -> p j c", c=65)
                pok = f"ps{4 + kh}"
                for ki, (kt, msk) in enumerate(keys):
                    sbk = ecnt % 2
                    bXY = [PS[2 * sbk], PS[2 * sbk + 1]]
                    for j in range(4):
                        h = kh * 4 + j; half = h % 2; hp = h // 2
                        C.mm(bXY[half][:, (j // 2) * 128:(j // 2 + 1) * 128], kT[half * 64:(half + 1) * 64, kh, kt * 128:(kt + 1) * 128],
                             qT[half * 64:(half + 1) * 64, hp, tq * 128:(tq + 1) * 128], True, True, allq + allk, [f"ps{2 * sbk + half}"])
                    E_ = E[sbk]; ek = f"w_E{sbk}"
                    Ev = E_[:, :].rearrange("p (jj two q) -> p two jj q", two=2, q=128)
                    for half in range(2):
                        C.act(Ev[:, half, :, :], bXY[half][:, 0:256].rearrange("p (jj q) -> p jj q", q=128), AF.Exp,
                              [f"ps{2 * sbk + half}"], [ek], scale=0.125, nosame=(half == 1))
                    if msk is not None:
                        C.tt("pool", E_[:, :], E_[:, :], mk[:, msk, :], ALU.mult, [ek, "w_mask"], [ek])
                    for j in range(4):
                        C.amm(4 + kh, pO[:, j, :], E_[:, j * 128:(j + 1) * 128], Vt[:, kt, kh, :], [ek, "w_V"], [pok])
                    ecnt += 1
                C.adone(4 + kh)
                C.tt("dve", d_[:, kh * 4:(kh + 1) * 4], pO[:, :, 64], sk[:, kh * 4:(kh + 1) * 4], ALU.add, [pok, "w_sink"], [dk])
                C.recip(d_[:, kh * 4:(kh + 1) * 4], d_[:, kh * 4:(kh + 1) * 4], [dk], [dk])
                C.tt("dve", y_[:, kh * 256:(kh + 1) * 256].rearrange("p (j d) -> p j d", d=64), pO[:, :, 0:64],
                     d_[:, kh * 4:(kh + 1) * 4].unsqueeze(2).to_broadcast([128, 4, 64]), ALU.mult, [pok, dk], [yk])
            emit_y(y_[:, :], 1, tq, yk)
        sb.release(m0)

    def na_keys(tq):
        n = tq - NCT
        r0 = 2 * n
        s0 = min(max(r0 - 4, 0), 24); s1 = min(max(r0 + 1 - 4, 0), 24)
        first = s0 // 2; last = (s1 + 7) // 2
        typ = 0 if n == 0 else 1 if n == 1 else 3 if n == 14 else 4 if n == 15 else 2
        return [(kt + NCT, kt - first) for kt in range(first, last + 1)], typ

    def phase_na(l):
        m0 = sb.mark()
        qT = sb.alloc("n_qT", [128, 4, T], BF16)
        kT = sb.alloc("n_kT", [128, 4, T], BF16)
        Vt = sb.alloc("n_V", [128, NT, 8, 65], BF16)
        load_v(3840, 8, 64, Vt, "n_V")
        prep_qk(l, 2816, 8, 4, False, qT, qscale=0.125, tag="n_q")
        prep_qk(l, 3328, 8, 5, False, kT, tag="n_k")
        alloc_y()
        nb = [sb.alloc(f"n_b{i}", [128, 8, 5, 128], BF16) for i in range(2)]
        E = [sb.alloc(f"n_E{i}", [128, 7, 128], BF16) for i in range(2)]
        ysb = [sb.alloc(f"n_y{i}", [128, 512], F32) for i in range(2)]
        den = [sb.alloc(f"n_den{i}", [128, 8], F32) for i in range(2)]
        allq = [("n_q", t) for t in range(NT)]; allk = [("n_k", t) for t in range(NT)]
        ecnt = 0
        for tq in range(NT):
            y_ = ysb[tq % 2]; yk = f"n_y{tq % 2}"
            d_ = den[tq % 2]; dk = f"n_den{tq % 2}"
            if tq < NCT:
                keys = [(0, None), (1, None)]
            else:
                loc, typ = na_keys(tq)
                keys = [(kt, sl) for kt, sl in loc] + [(0, None), (1, None)]
                b_ = nb[tq % 2]; bk = f"n_b{tq % 2}"
                p.dma("pool", [lambda e, b_=b_, typ=typ: e.dma_start(
                    out=b_[:, :, :, :], in_=I["nab"][l, typ, :, :].rearrange("p (h s k) -> p h s k", h=8, s=5))], bk, [], [bk])
            nk = len(keys)
            for h in range(8):
                half = h % 2; hp = h // 2
                set_ = ecnt % 2
                bA = PS[2 * set_]; bB = PS[2 * set_ + 1]
                for i_, (kt, sl) in enumerate(keys):
                    bank = bA if i_ < 4 else bB
                    bkey = f"ps{2 * set_ + (0 if i_ < 4 else 1)}"
                    reg = bank[:, (i_ % 4) * 128:(i_ % 4 + 1) * 128]
                    C.mm(reg, kT[half * 64:(half + 1) * 64, hp, kt * 128:(kt + 1) * 128],
                         qT[half * 64:(half + 1) * 64, hp, tq * 128:(tq + 1) * 128], True, sl is None, allq + allk, [bkey])
                    if sl is not None:
                        C.mm(reg, b_[:, h, sl, :], identb[:, :], False, True, [bk, "identb"], [bkey])
                E_ = E[set_]; ek = f"n_E{set_}"
                nA = min(4, nk)
                C.act(E_[:, 0:nA, :], bA[:, 0:nA * 128].rearrange("p (a b) -> p a b", b=128), AF.Exp, [f"ps{2 * set_}"], [ek])
                if nk > 4:
                    C.act(E_[:, 4:nk, :], bB[:, 0:(nk - 4) * 128].rearrange("p (a b) -> p a b", b=128), AF.Exp,
                          [f"ps{2 * set_ + 1}"], [ek], nosame=True)
                pob = 4 + (h // 4)
                pO = PS[pob][:, 0:260].rearrange("p (j c) -> p j c", c=65)
                for i_, (kt, sl) in enumerate(keys):
                    C.amm(pob, pO[:, h % 4, :], E_[:, i_, :], Vt[:, kt, h, :], [ek, "n_V"], [f"ps{pob}"])
                ecnt += 1
                if h % 4 == 3:
                    C.adone(pob)
                    g = h // 4
                    C.recip(d_[:, g * 4:(g + 1) * 4], pO[:, :, 64], [f"ps{pob}"], [dk])
                    C.tt("dve", y_[:, g * 256:(g + 1) * 256].rearrange("p (j d) -> p j d", d=64), pO[:, :, 0:64],
                         d_[:, g * 4:(g + 1) * 4].unsqueeze(2).to_broadcast([128, 4, 64]), ALU.mult, [f"ps{pob}", dk], [yk])
            emit_y(y_[:, :], 3, tq, yk)
        sb.release(m0)

    def phase_diff(l, lam_init):
        m0 = sb.mark()
        qT = sb.alloc("d_qT", [128, 4, T], BF16)
        kT = sb.alloc("d_kT", [128, 4, T], BF16)
        Vt = sb.alloc("d_V", [128, NT, 4, 129], BF16)
        load_v(2304, 4, 128, Vt, "d_V")
        lp = sb.alloc("d_lp", [128, 4, 64], F32)
        lw = sb.alloc("d_lw", [128, 8], F32)
        sg = sb.alloc("d_sg", [128, 128], F32)
        bcast_load(lp[:, :, :].rearrange("p a d -> p (a d)"), I["dlam"][l, :], 256, "d_lp", "d_lp")
        bcast_load(sg[:, :], I["subg"][l, :], 128, "d_sg", "d_sg")
        C.ts("dve", sg[:, :], sg[:, :], 1.0 - lam_init, None, ALU.mult, None, ["d_sg"], ["d_sg"])
        C.tt("dve", lp[:, 0, :], lp[:, 0, :], lp[:, 1, :], ALU.mult, ["d_lp"], ["d_lp"])
        C.tt("dve", lp[:, 2, :], lp[:, 2, :], lp[:, 3, :], ALU.mult, ["d_lp"], ["d_lp"])
        C.red(lw[:, 0:1], lp[:, 0, :], ALU.add, ["d_lp"], ["d_lw"])
        C.red(lw[:, 1:2], lp[:, 2, :], ALU.add, ["d_lp"], ["d_lw"])
        C.act(lw[:, 2:4], lw[:, 0:2], AF.Exp, ["d_lw"], ["d_lw"])
        C.tt("dve", lw[:, 4:5], lw[:, 3:4], lw[:, 2:3], ALU.subtract, ["d_lw"], ["d_lw"])
        C.ts("dve", lw[:, 5:6], lw[:, 4:5], -lam_init, None, ALU.add, None, ["d_lw"], ["d_lw"])
        prep_qk(l, 1280, 8, 2, True, qT, tag="d_q")
        prep_qk(l, 1792, 8, 3, True, kT, tag="d_k")
        alloc_y()
        E = [sb.alloc(f"d_E{i}", [128, 512], BF16) for i in range(2)]
        ybuf = sb.alloc("d_ybuf", [128, 4, 512], F32)
        o1 = [sb.alloc(f"d_o1{i}", [128, 128], F32) for i in range(2)]
        junk = sb.alloc("d_junk", [128, 128], F32)
        dst_ = [sb.alloc(f"d_st{i}", [128, 8], F32) for i in range(2)]
        allq = [("d_q", t) for t in range(NT)]; allk = [("d_k", t) for t in range(NT)]
        chunks = [[0, 1]] + [[2 + 4 * c + j for j in range(4)] for c in range(4)]
        ecnt = 0; ocnt = 0
        def preg(m, j):
            r = m * 4 + j
            return PS[4 + r // 3][:, (r % 3) * 129:(r % 3 + 1) * 129], f"ps{4 + r // 3}"
        for ch in chunks:
            keys = [0, 1] if ch[0] < NCT else list(range(NT))
            nq = len(ch) * 128
            c0 = ch[0] * 128
            for h in range(4):
                for m in range(2):
                    for ki, kt in enumerate(keys):
                        sbk = ecnt % 2
                        bank = PS[sbk]
                        C.mm(bank[:, 0:nq], kT[m * 64:(m + 1) * 64, h, kt * 128:(kt + 1) * 128],
                             qT[m * 64:(m + 1) * 64, h, c0:c0 + nq], True, True, allq + allk, [f"ps{sbk}"])
                        E_ = E[sbk]; ek = f"d_E{sbk}"
                        C.act(E_[:, 0:nq], bank[:, 0:nq], AF.Exp, [f"ps{sbk}"], [ek], scale=0.125)
                        for j in range(len(ch)):
                            reg, rk = preg(m, j)
                            C.amm(int(rk[2:]), reg, E_[:, j * 128:(j + 1) * 128], Vt[:, kt, h, :], [ek, "d_V"], [rk])
                        ecnt += 1
                for bnk in (4, 5, 6):
                    C.adone(bnk)
                for j in range(len(ch)):
                    r0_, k0 = preg(0, j); r1_, k1 = preg(1, j)
                    st_ = dst_[ocnt % 2]; sk_ = f"d_st{ocnt % 2}"
                    o_ = o1[ocnt % 2]; ok_ = f"d_o1{ocnt % 2}"
                    C.recip(st_[:, 0:1], r0_[:, 128:129], [k0], [sk_])
                    C.recip(st_[:, 1:2], r1_[:, 128:129], [k1], [sk_])
                    C.tt("dve", st_[:, 2:3], st_[:, 1:2], lw[:, 5:6], ALU.mult, [sk_, "d_lw"], [sk_])
                    C.ts("dve", o_[:, :], r0_[:, 0:128], st_[:, 0:1], None, ALU.mult, None, [k0, sk_], [ok_])
                    C.stt(o_[:, :], r1_[:, 0:128], st_[:, 2:3], o_[:, :], ALU.mult, ALU.add, [k1, sk_, ok_], [ok_])
                    C.act(junk[:, :], o_[:, :], AF.Square, [ok_], ["d_junk", sk_], accum=st_[:, 3:4])
                    C.ts("dve", st_[:, 4:5], st_[:, 3:4], 1.0 / 128, EPS, ALU.mult, ALU.add, [sk_], [sk_])
                    C.act(st_[:, 5:6], st_[:, 4:5], AF.Sqrt, [sk_], [sk_])
                    C.recip(st_[:, 6:7], st_[:, 5:6], [sk_], [sk_])
                    C.stt(ybuf[:, j, h * 128:(h + 1) * 128], o_[:, :], st_[:, 6:7], sg[:, :], ALU.mult, ALU.mult,
                          [ok_, sk_, "d_sg"], [("d_ybuf", j)])
                    ocnt += 1
            for j, tq in enumerate(ch):
                emit_y(ybuf[:, j, :], 2, tq, ("d_ybuf", j))
        sb.release(m0)

    def phase_s5(l):
        NK = 288
        KT = [(0, 128), (128, 128), (256, 32)]
        mP = sb.mark()
        GL = sb.alloc("s5_GL", [128, 32, 2, 128], BF16)
        KS = sb.alloc("s5_KS", [128, 32, 128], BF16)
        HH = sb.alloc("s5_HH", [128, 32, 2, 128], BF16)
        A8 = sb.alloc("s5_A8", [128, 2, 2, 32], F32)
        mS = sb.mark()
        a = sb.alloc("s5_a", [128, 32, 2], F32)
        w = sb.alloc("s5_w", [128, 24, 32], F32)
        Bm = sb.alloc("s5_B", [128, 32, 2, 16], F32)
        Cm = sb.alloc("s5_C", [128, 32, 2, 16], F32)
        Bb = sb.alloc("s5_Bb", [128, 2, 32, 16], F32)
        Pp = sb.alloc("s5_Pp", [128, 2, 32, 9], F32)
        Pn = sb.alloc("s5_Pn", [128, 2, 32, 8], F32)
        PG = sb.alloc("s5_PG", [128, 3, 2, 32, 8], F32)
        Gf = sb.alloc("s5_Gf", [128, 2, 32, 128], F32)
        Rf = sb.alloc("s5_Rf", [128, 2, 32, 128], F32)
        t1 = sb.alloc("s5_t1", [128, 32, 128], F32)
        t2 = sb.alloc("s5_t2", [128, 32, 128], F32)
        msk = sb.alloc("s5_msk", [128, 2, 128], F32)
        dd = sb.alloc("s5_dd", [128, 32], F32)
        K = "s5set"
        C.dma("sp", a[:, :, :], I["s5a"][l], "s5_a", [], [K])
        C.dma("sp", w[:, 0, :], I["s5ls"][l], "s5_ls", [], [K])
        C.dma("sp", Bm[:, :, :, :], I["s5b"][l], "s5_b", [], [K])
        C.dma("sp", Cm[:, :, :, :], I["s5c"][l], "s5_c", [], [K])
        C.dma("sp", dd[:, :], I["s5d"][l], "s5_d", [], [K])
        C.dma("sp", msk[:, :, :], I["s5mask"].rearrange("m p c -> p m c"), "s5_m", [], [K])
        W = lambda i: w[:, i, :]
        are = a[:, :, 0]; aim = a[:, :, 1]
        def TT(o, x, y, op): C.tt("dve", o, x, y, op, [K], [K])
        def TS(o, x, s1, s2, op0, op1): C.ts("dve", o, x, s1, s2, op0, op1, [K], [K])
        def ACT(o, x, f, scale=1.0): C.act(o, x, f, [K], [K], scale=scale)
        ACT(W(0), W(0), AF.Exp)
        TT(W(1), are, W(0), ALU.mult)
        TT(W(2), aim, W(0), ALU.mult)
        ACT(W(3), W(1), AF.Exp)
        ACT(W(4), W(1), AF.Exp, scale=-1.0)
        ACT(W(5), W(2), AF.Sin, scale=1.0 / 32)
        ACT(W(6), W(2), AF.Sin, scale=1.0 / 16)
        TT(W(7), W(5), W(5), ALU.mult)
        TS(W(7), W(7), -2.0, 1.0, ALU.mult, ALU.add)
        for _ in range(4):
            TT(W(8), W(7), W(7), ALU.mult)
            TT(W(9), W(6), W(6), ALU.mult)
            TT(W(10), W(7), W(6), ALU.mult)
            TT(W(7), W(8), W(9), ALU.subtract)
            TS(W(6), W(10), 2.0, None, ALU.mult, None)
        Lr, Li, Ir, Ii = W(11), W(12), W(13), W(14)
        TT(Lr, W(3), W(7), ALU.mult); TT(Li, W(3), W(6), ALU.mult)
        TT(Ir, W(4), W(7), ALU.mult); TT(Ii, W(4), W(6), ALU.mult)
        TS(Ii, Ii, -1.0, None, ALU.mult, None)
        TS(W(15), Lr, -1.0, None, ALU.add, None)
        TT(W(16), W(15), are, ALU.mult); TT(W(17), Li, aim, ALU.mult); TT(W(16), W(16), W(17), ALU.add)
        TT(W(17), Li, are, ALU.mult); TT(W(18), W(15), aim, ALU.mult); TT(W(17), W(17), W(18), ALU.subtract)
        TT(W(18), are, are, ALU.mult); TT(W(19), aim, aim, ALU.mult); TT(W(18), W(18), W(19), ALU.add)
        p.op("dve", lambda e: e.reciprocal(out=W(18), in_=W(18)), [K], [K])
        TT(W(16), W(16), W(18), ALU.mult); TT(W(17), W(17), W(18), ALU.mult)
        gr = W(16).unsqueeze(2).to_broadcast([128, 32, 16]); gi = W(17).unsqueeze(2).to_broadcast([128, 32, 16])
        Br = Bm[:, :, 0, :]; Bi = Bm[:, :, 1, :]
        T1 = t1[:, :, 0:16]; T2 = t2[:, :, 0:16]
        TT(T1, Br, gr, ALU.mult); TT(T2, Bi, gi, ALU.mult); TT(Bb[:, 0, :, :], T1, T2, ALU.subtract)
        TT(T1, Bi, gr, ALU.mult); TT(T2, Br, gi, ALU.mult); TT(Bb[:, 1, :, :], T1, T2, ALU.add)
        C.memset("dve", Pp[:, 0, :, 0:1], 1.0, [K]); C.memset("dve", Pp[:, 1, :, 0:1], 0.0, [K])
        C.memset("dve", Pn[:, 0, :, 0:1], 1.0, [K]); C.memset("dve", Pn[:, 1, :, 0:1], 0.0, [K])
        def cpow(P, n, Xr, Xi):
            for s_ in range(1, n):
                pr, pi = P[:, 0, :, s_ - 1], P[:, 1, :, s_ - 1]
                TT(W(20), pr, Xr, ALU.mult); TT(W(21), pi, Xi, ALU.mult); TT(P[:, 0, :, s_], W(20), W(21), ALU.subtract)
                TT(W(20), pr, Xi, ALU.mult); TT(W(21), pi, Xr, ALU.mult); TT(P[:, 1, :, s_], W(20), W(21), ALU.add)
        cpow(Pp, 9, Lr, Li)
        cpow(Pn, 8, Ir, Ii)
        for ri_ in range(2):
            C.cp("dve", A8[:, 0, ri_, :], Pp[:, 0, :, 8], [K], [K])
        TS(A8[:, 1, 0, :], Pp[:, 1, :, 8], -1.0, None, ALU.mult, None)
        C.cp("dve", A8[:, 1, 1, :], Pp[:, 1, :, 8], [K], [K])
        for ri_ in range(2):
            C.cp("dve", PG[0:64, 0, ri_, :, :], Pp[0:64, ri_, :, 7::-1], [K], [K])
            C.cp("dve", PG[64:128, 0, ri_, :, :], Pp[64:128, ri_, :, 0:8], [K], [K])
            C.cp("dve", PG[0:64, 1, ri_, :, :], Pp[0:64, ri_, :, 1:9], [K], [K])
            C.cp("dve", PG[64:128, 1, ri_, :, :], Pp[64:128, ri_, :, 8:0:-1], [K], [K])
            C.cp("dve", PG[0:64, 2, ri_, :, :], Pn[0:64, ri_, :, 7::-1], [K], [K])
            C.cp("dve", PG[64:128, 2, ri_, :, :], Pn[64:128, ri_, :, 0:8], [K], [K])
        def outer(dst_r, dst_i, tb, Mr, Mi, neg_i):
            pr = PG[:, tb, 0, :, :].unsqueeze(3).to_broadcast([128, 32, 8, 16])
            pi = PG[:, tb, 1, :, :].unsqueeze(3).to_broadcast([128, 32, 8, 16])
            mr = Mr.unsqueeze(2).to_broadcast([128, 32, 8, 16]); mi = Mi.unsqueeze(2).to_broadcast([128, 32, 8, 16])
            A_ = t1[:, :, :].rearrange("p g (s c) -> p g s c", c=16); B_ = t2[:, :, :].rearrange("p g (s c) -> p g s c", c=16)
            TT(A_, pr, mr, ALU.mult); TT(B_, pi, mi, ALU.mult); TT(dst_r, A_, B_, ALU.subtract)
            TT(A_, pr, mi, ALU.mult); TT(B_, pi, mr, ALU.mult)
            TT(dst_i, A_, B_, ALU.add)
            if neg_i:
                TS(dst_i, dst_i, -1.0, None, ALU.mult, None)
        v4 = lambda x: x.rearrange("p g (s c) -> p g s c", c=16)
        outer(v4(Gf[:, 0, :, :]), v4(Gf[:, 1, :, :]), 0, Bb[:, 0, :, :], Bb[:, 1, :, :], False)
        outer(v4(Rf[:, 0, :, :]), v4(Rf[:, 1, :, :]), 2, Cm[:, :, 0, :], Cm[:, :, 1, :], True)
        for g in range(32):
            pb = g % 2
            bank = PS[pb]; bk = f"ps{pb}"
            bankb = PS[4 + pb]; bkb = f"ps{4 + pb}"
            for d_ in range(2):
                sl = slice(64 * d_, 64 * (d_ + 1))
                reg = (bank if d_ == 0 else bankb)[:, 0:128]
                C.mm(reg, Gf[sl, 0, g, :], Rf[sl, 0, g, :], True, False, [K], [bk if d_ == 0 else bkb])
                C.mm(reg, Gf[sl, 1, g, :], Rf[sl, 1, g, :], False, True, [K], [bk if d_ == 0 else bkb])
            ks = t1[:, g, :]
            C.tt("dve", ks, bank[:, 0:128], msk[:, 0, :], ALU.mult, [bk, K], [K])
            C.tt("dve", t2[:, g, :], bankb[:, 0:128], msk[:, 1, :], ALU.mult, [bkb, K], [K])
            C.tt("dve", ks, ks, t2[:, g, :], ALU.add, [K], [K])
            C.stt(KS[:, g, :], ident[:, :], dd[:, g:g + 1], ks, ALU.mult, ALU.add, ["ident", K], ["s5_KS", K])
            pb2 = 2 + g % 2
            pT = PS[pb2][:, 0:256].rearrange("p (a b) -> p a b", b=128)
            for ri_ in range(2):
                C.tr(pT[:, ri_, :], Gf[:, ri_, g, :], ident[:, :], [K, "ident"], [f"ps{pb2}"])
            C.act(GL[:, g, :, :], pT[:, :, :], AF.Copy, [f"ps{pb2}"], ["s5_GL"])
        Hs = Rf
        outer(v4(Hs[:, 0, :, :]), v4(Hs[:, 1, :, :]), 1, Cm[:, :, 0, :], Cm[:, :, 1, :], True)
        for ri_ in range(2):
            C.cp("dve", HH[:, :, ri_, :], Hs[:, ri_, :, :], [K], ["s5_HH"])
        sb.release(mS)
        uB = sb.alloc("s5_uB", [128, 32, NK], BF16)
        mS = sb.mark()
        ubs = sb.alloc("s5_ubs", [128, 8, 512], F32)
        ubp = sb.alloc("s5_ubp", [128, 32, 128], F32)
        for (k0, kn) in KT:
            C.dma("sp", ubs[0:kn, :, :], PROJ[8 * k0:8 * (k0 + kn), 0:512].rearrange("(k t) c -> k t c", t=8), "s5_ubs",
                  [("PROJ", t_, c_) for t_ in range(NT) for c_ in range(9)], ["s5_ubs"])
            C.cp("dve", ubp[0:kn, :, :].rearrange("k g (t c) -> k g t c", c=16),
                 ubs[0:kn, :, :].rearrange("k t (g c) -> k g t c", c=16), ["s5_ubs"], ["s5_ubp"])
            for g4 in range(8):
                pb = 2 + g4 % 2
                pT = PS[pb][:, :].rearrange("p (a b) -> p a b", b=128)
                for a_ in range(4):
                    g = g4 * 4 + a_
                    C.tr(pT[:, a_, 0:kn], ubp[0:kn, g, :], ident[0:kn, 0:kn], ["s5_ubp", "ident"], [f"ps{pb}"])
                C.act(uB[:, g4 * 4:(g4 + 1) * 4, k0:k0 + kn], pT[:, :, 0:kn], AF.Copy, [f"ps{pb}"], ["s5_uB"], nosame=True)
        sb.release(mS)
        X = sb.alloc("s5_X", [128, 2, 32, NK], F32)
        for g in range(32):
            pb = g % 2
            for ri_ in range(2):
                C.mm(PS[2 * pb + ri_][:, 0:NK], GL[:, g, ri_, :], uB[:, g, :], True, True, ["s5_GL", "s5_uB"], [f"ps{2 * pb + ri_}"])
                C.cp("act" if ri_ == 0 else "dve", X[:, ri_, g, :], PS[2 * pb + ri_][:, 0:NK], [f"ps{2 * pb + ri_}"], ["s5_X0"])
        sT = [sb.alloc(f"s5_sT{i}", [128, 2, 2, 32], F32) for i in range(1)][0]
        def step(eng, sl, k, kp, key):
            Z = X[sl, :, :, kp]; Zs = X[sl, ::-1, :, kp]
            C.tt(eng, sT[sl, 0, :, :], A8[sl, 0, :, :], Z, ALU.mult, [key, "s5_X0", K], [key + "a"])
            C.tt(eng, sT[sl, 1, :, :], A8[sl, 1, :, :], Zs, ALU.mult, [key, "s5_X0", K], [key + "b"])
            C.tt(eng, sT[sl, 0, :, :], sT[sl, 0, :, :], sT[sl, 1, :, :], ALU.add, [key + "a", key + "b"], [key + "a"])
            C.tt(eng, X[sl, :, :, k], X[sl, :, :, k], sT[sl, 0, :, :], ALU.add, [key + "a", key, "s5_X0"], [key])
        for k in range(1, NK):
            step("dve", slice(0, 64), k, k - 1, "s5_Xf")
        border = [(k, k + 1) for k in range(30, -1, -1)] + [(287, 0)] + [(k, k + 1) for k in range(286, 31, -1)]
        for (k, kp) in border:
            step("pool", slice(64, 128), k, kp, "s5_Xb")
        XpB = sb.alloc("s5_XpB", [128, 2, 32, NK], BF16)
        xr = ["s5_Xf", "s5_Xb", "s5_X0"]
        C.memset("dve", XpB[0:64, :, :, 0:1], 0.0, ["s5_XpBf"])
        C.cp("dve", XpB[0:64, :, :, 1:NK], X[0:64, :, :, 0:NK - 1], xr, ["s5_XpBf"])
        C.cp("pool", XpB[64:128, :, :, 0:NK - 1], X[64:128, :, :, 1:NK], xr, ["s5_XpBb"])
        C.memset("pool", XpB[64:128, :, :, 31:32], 0.0, ["s5_XpBb"])
        C.cp("pool", XpB[64:128, :, :, NK - 1:NK], X[64:128, :, :, 0:1], xr + ["s5_XpBb"], ["s5_XpBb"])
        yt_ = sb.alloc("s5_yt", [128, 8, 32, 16], F32)
        cnt = 0
        for (k0, kn) in KT:
            for g4 in range(8):
                pb = cnt % 2; cnt += 1
                for a_ in range(4):
                    g = g4 * 4 + a_
                    reg = PS[pb][0:kn, a_ * 128:(a_ + 1) * 128]
                    C.mm(reg, uB[:, g, k0:k0 + kn], KS[:, g, :], True, False, ["s5_uB", "s5_KS"], [f"ps{pb}"])
                    C.mm(reg, XpB[:, 0, g, k0:k0 + kn], HH[:, g, 0, :], False, False, ["s5_XpBf", "s5_XpBb", "s5_HH"], [f"ps{pb}"])
                    C.mm(reg, XpB[:, 1, g, k0:k0 + kn], HH[:, g, 1, :], False, True, ["s5_XpBf", "s5_XpBb", "s5_HH"], [f"ps{pb}"])
                C.cp("act" if g4 % 2 else "dve", yt_[0:kn, :, g4 * 4:(g4 + 1) * 4, :].rearrange("k i g c -> k g i c"),
                     PS[pb][0:kn, :].rearrange("k (g i c) -> k g i c", i=8, c=16), [f"ps{pb}"], ["s5_yt"], )
            C.dma("sp", YS[8 * k0:8 * (k0 + kn), :].rearrange("(k i) c -> k i c", i=8), yt_[0:kn, :, :, :].rearrange("k i g c -> k i (g c)"),
                  "s5_yt", ["s5_yt"], [("YS", k0)])
        sb.release(mP)
        wg = sb.alloc("s5_wg", [128, 4, 512], BF16)
        bg = sb.alloc("s5_bg", [128, 512], F32)
        p.dma("pool", [lambda e: e.dma_start(out=wg[:, :, :], in_=I["s5_w_glu"][l].rearrange("(kc p) n -> p kc n", p=128))],
              "s5_wg", [], ["s5_wg"])
        bcast_load(bg[:, :], I["s5_b_glu"][l, :], 512, "s5_bg", "s5_bg")
        alloc_y()
        ysb = [sb.alloc(f"s5_y{i}", [128, 512], F32) for i in range(2)]
        ug = [sb.alloc(f"s5_u{i}", [128, 512], F32) for i in range(2)]
        ygb = [sb.alloc(f"s5_ygb{i}", [128, 512], BF16) for i in range(2)]
        ygT = [sb.alloc(f"s5_ygT{i}", [128, 4, 128], BF16) for i in range(2)]
        ysall = [("YS", k0) for (k0, kn) in KT]
        for tt in range(NT):
            i = tt % 2
            y_, u_, gb_, gT_ = ysb[i], ug[i], ygb[i], ygT[i]
            ky, ku, kg, kt_ = f"s5_y{i}", f"s5_u{i}", f"s5_ygb{i}", f"s5_ygT{i}"
            C.dma("sp", y_[:, :], YS[tt * 128:(tt + 1) * 128, :], ky, ysall, [ky])
            C.tt("dve", u_[:, :], y_[:, :], y_[:, :], ALU.mult, [ky], [ku])
            C.ts("dve", u_[:, :], u_[:, :], 0.044715, 1.0, ALU.mult, ALU.add, [ku], [ku])
            C.tt("dve", u_[:, :], u_[:, :], y_[:, :], ALU.mult, [ku, ky], [ku])
            C.act(u_[:, :], u_[:, :], AF.Sigmoid, [ku], [ku], scale=2.0 * math.sqrt(2.0 / math.pi))
            C.tt("dve", y_[:, :], y_[:, :], u_[:, :], ALU.mult, [ku, ky], [ky])
            C.cp("dve", gb_[:, :], y_[:, :], [ky], [kg])
            pTb = PS[7][:, :].bitcast(BF16).rearrange("p (a b) -> p a b", b=128)
            for a_ in range(4):
                C.tr(pTb[:, a_, :], gb_[:, a_ * 128:(a_ + 1) * 128], identb[:, :], [kg, "identb"], ["ps7"])
            C.cp("act", gT_[:, :, :], pTb[:, 0:4, :], ["ps7"], [kt_])
            pb = i
            for kc in range(4):
                C.mm(PS[pb][:, :], gT_[:, kc, :], wg[:, kc, :], kc == 0, kc == 3, [kt_, "s5_wg"], [f"ps{pb}"])
            C.tt("dve", u_[:, :], PS[pb][:, :], bg[:, :], ALU.add, [f"ps{pb}", "s5_bg", ku], [ku])
            C.act(u_[:, :], u_[:, :], AF.Sigmoid, [ku], [ku])
            C.tt("dve", y_[:, :], y_[:, :], u_[:, :], ALU.mult, [ku, ky], [ky])
            emit_y(y_[:, :], 0, tt, ky)
        sb.release(mP)

    TCH = [(0, 512), (512, 512), (1024, 512), (1536, 512), (2048, 256)]

    def build_gateB(gB, j):
        dg = [sb.alloc(f"gdiag{i}", [128, 128], F32) for i in range(2)]
        cnt = 0
        for lc in range(2):
            for k4 in range(4):
                pb = 2 + k4 % 2
                for a_ in range(4):
                    kc = k4 * 4 + a_
                    d_ = dg[cnt % 2]; dk = f"gdiag{cnt % 2}"; cnt += 1
                    C.ts("dve", d_[:, :], ident[:, :], modT[:, j * 16 + kc, lc:lc + 1], None, ALU.mult, None, ["ident", "modT"], [dk])
                    C.mm(PS[pb][:, a_ * 128:(a_ + 1) * 128], ones[:, :], d_[:, :], True, True, [dk, "ones"], [f"ps{pb}"])
                C.cp("act", gB[:, lc, k4 * 512:(k4 + 1) * 512], PS[pb][:, :], [f"ps{pb}"], ["gateB"], )

    def phase_merge(l):
        m0 = sb.mark()
        wbr = sb.alloc("wbr", [128, 16, 2048], BF16)
        wsrc = I["w_branch"][l].rearrange("(k p) n -> p k n", p=128)
        for k4 in range(4):
            p.dma("pool", [lambda e, k4=k4: e.dma_start(out=wbr[:, k4 * 4:(k4 + 1) * 4, :], in_=wsrc[:, k4 * 4:(k4 + 1) * 4, :])],
                  "wbr", [], ["wbr"])
        ytc = [sb.alloc(f"ytc{i}", [128, 16, 512], BF16) for i in range(2)]
        sgb = [sb.alloc(f"sgb{i}", [128, 4, 512], BF16) for i in range(2)]
        acc = [sb.alloc(f"macc{i}", [128, 512], F32) for i in range(2)]
        tmp = [sb.alloc(f"mtmp{i}", [128, 512], F32) for i in range(2)]
        mgo = [sb.alloc(f"mgo{i}", [128, 512], BF16) for i in range(2)]
        SGv = SG.rearrange("(i f) p t -> f p i t", i=4)
        yall = [("YT", b, t) for b in range(4) for t in range(NT)]
        sgall = [("SG", gi) for gi in range(64)]
        cnt = 0; fcnt = 0
        for ci, (t0, tw) in enumerate(TCH):
            y_ = ytc[ci % 2]; yk = f"ytc{ci % 2}"
            C.dma("sp", y_[:, :, 0:tw], YT[:, :, t0:t0 + tw].rearrange("k p t -> p k t"), yk, yall, [yk])
            for fc in range(16):
                s_ = sgb[fcnt % 2]; sk = f"sgb{fcnt % 2}"
                a_ = acc[fcnt % 2]; ak = f"macc{fcnt % 2}"
                o_ = mgo[fcnt % 2]; ok_ = f"mgo{fcnt % 2}"
                fcnt += 1
                C.dma("sp", s_[:, :, 0:tw], SGv[fc][:, :, t0:t0 + tw], sk, sgall, [sk])
                for i in range(4):
                    pb = cnt % 2
                    t_ = tmp[cnt % 2]; tk = f"mtmp{cnt % 2}"
                    cnt += 1
                    for kc in range(4):
                        C.mm(PS[pb][:, 0:tw], wbr[:, i * 4 + kc, fc * 128:(fc + 1) * 128], y_[:, i * 4 + kc, 0:tw], kc == 0, kc == 3,
                             ["wbr", yk], [f"ps{pb}"])
                    if i == 0:
                        C.tt("dve", a_[:, 0:tw], PS[pb][:, 0:tw], s_[:, i, 0:tw], ALU.mult, [f"ps{pb}", sk], [ak])
                    else:
                        C.tt("dve", t_[:, 0:tw], PS[pb][:, 0:tw], s_[:, i, 0:tw], ALU.mult, [f"ps{pb}", sk], [tk])
                        if i < 3:
                            C.tt("pool", a_[:, 0:tw], a_[:, 0:tw], t_[:, 0:tw], ALU.add, [ak, tk], [ak])
                        else:
                            C.tt("pool", o_[:, 0:tw], a_[:, 0:tw], t_[:, 0:tw], ALU.add, [ak, tk], [ok_])
                C.dma("sp", MG[fc, :, t0:t0 + tw], o_[:, 0:tw], ok_, [ok_], [("MG", fc, ci)])
        sb.release(m0)

    def resid_update(src_ap, gB, tt, nb, srckeys, evs, cnt):
        lc = tok_lc(tt)
        e_ = evs[cnt % 2]; ek = f"rev{cnt % 2}"
        C.tt("dve", e_[:, :], src_ap, gB[:, lc, nb * 512:(nb + 1) * 512], ALU.mult, srckeys + ["gateB"], [ek])
        p.dma("pool", [lambda e: e.dma_start(out=XR[tt * 128:(tt + 1) * 128, nb * 512:(nb + 1) * 512], in_=e_[:, :], accum_op=ALU.add)],
              ek, [ek, ("XR", tt)], [("XR", tt)])

    def phase_wout(l):
        m0 = sb.mark()
        wo = sb.alloc("wo", [128, 16, 2048], BF16)
        wsrc = I["w_out"][l].rearrange("(k p) n -> p k n", p=128)
        for k4 in range(4):
            p.dma("pool", [lambda e, k4=k4: e.dma_start(out=wo[:, k4 * 4:(k4 + 1) * 4, :], in_=wsrc[:, k4 * 4:(k4 + 1) * 4, :])],
                  "wo", [], ["wo"])
        gB = sb.alloc("gateB", [128, 2, 2048], F32)
        build_gateB(gB, 2)
        mgt = [sb.alloc(f"mgt{i}", [128, 16, 128], BF16) for i in range(2)]
        evs = [sb.alloc(f"rev{i}", [128, 512], F32) for i in range(2)]
        mgall = [("MG", fc, ci) for fc in range(16) for ci in range(5)]
        cnt = 0
        for tt in range(NT):
            m_ = mgt[tt % 2]; mk_ = f"mgt{tt % 2}"
            C.dma("sp", m_[:, :, :], MG[:, :, tt * 128:(tt + 1) * 128].rearrange("k p t -> p k t"), mk_, mgall, [mk_])
            for nb in range(4):
                pb = cnt % 2
                for fc in range(16):
                    C.mm(PS[pb][:, :], m_[:, fc, :], wo[:, fc, nb * 512:(nb + 1) * 512], fc == 0, fc == 15, [mk_, "wo"], [f"ps{pb}"])
                resid_update(PS[pb][:, :], gB, tt, nb, [f"ps{pb}"], evs, cnt)
                cnt += 1
        sb.release(m0)

    def phase_router_norm(l, hT):
        m0 = sb.mark()
        xt = [sb.alloc(f"xt{i}", [128, D], F32) for i in range(2)]
        junk = sb.alloc("junk", [128, D], BF16)
        st = [sb.alloc(f"nst{i}", [128, 4], F32) for i in range(2)]
        hf = [sb.alloc(f"hf{i}", [128, 16, 128], F32) for i in range(2)]
        wr = sb.alloc("wr", [128, 16, 20], F32)
        br = sb.alloc("br", [128, 20], F32)
        cw = sb.alloc("cw", [128, NT, 16], F32)
        rt = [sb.alloc(f"rt{i}", [128, 96], F32) for i in range(2)]
        C.dma("sp", wr[:, :, :], I["wrT"][l].rearrange("p (k n) -> p k n", n=20), "wr", [], ["wr"])
        bcast_load(br[:, :], I["brt"][l, :], 20, "br", "br")
        s = 1
        for tt in range(NT):
            i = tt % 2
            lc = tok_lc(tt)
            x_, s_, h_ = xt[i], st[i], hf[i]
            hk = f"hf{i}"
            C.dma("sp", x_[:, :], XR[tt * 128:(tt + 1) * 128, :], f"xt{i}", [("XR", tt)], [f"xt{i}"])
            C.act(junk[:, :], x_[:, :], AF.Square, [f"xt{i}"], ["junk", f"nst{i}a"], accum=s_[:, 0:1])
            C.ts("dve", s_[:, 1:2], s_[:, 0:1], 1.0 / D, EPS, ALU.mult, ALU.add, [f"nst{i}a"], [f"nst{i}b"])
            C.act(s_[:, 2:3], s_[:, 1:2], AF.Sqrt, [f"nst{i}b"], [f"nst{i}c"])
            C.recip(s_[:, 3:4], s_[:, 2:3], [f"nst{i}c"], [f"nst{i}d"])
            C.ts("dve", x_[:, :], x_[:, :], s_[:, 3:4], None, ALU.mult, None, [f"xt{i}", f"nst{i}d"], [f"xt{i}"])
            for g4 in range(4):
                pb = 2 + (g4 % 2)
                pT = PS[pb][:, :].rearrange("p (a b) -> p a b", b=128)
                for a in range(4):
                    kc = g4 * 4 + a
                    C.tr(pT[:, a, :], x_[:, kc * 128:(kc + 1) * 128], ident[:, :], [f"xt{i}", "ident"], [f"ps{pb}"])
                for a in range(4):
                    kc = g4 * 4 + a
                    p.op("dve", lambda e, kc=kc, a=a, pT=pT, h_=h_, lc=lc: e.tensor_scalar(
                        out=h_[:, kc, :], in0=pT[:, a, :], scalar1=Amod[:, s, kc, lc:lc + 1], scalar2=modT[:, (3 * s) * 16 + kc, lc:lc + 1],
                        op0=ALU.mult, op1=ALU.add), [f"ps{pb}", "Amod", "modT"], [hk], nosame=True)
            C.cp("act", hT[:, :, tt * 128:(tt + 1) * 128], h_[:, :, :], [hk], [("hT", tt)])
            for kc in range(16):
                C.mm(PS[7][:, 0:20], h_[:, kc, :], wr[:, kc, :], kc == 0, kc == 15, [hk, "wr"], ["ps7"])
            r_ = rt[i]; rk = f"rt{i}"
            lg = r_[:, 0:20]
            C.tt("dve", lg, PS[7][:, 0:20], br[:, :], ALU.add, ["ps7", "br"], [rk])
            def R(o, x, y, op): C.tt("dve", o, x, y, op, [rk], [rk])
            def RS(o, x, s1, s2, op0, op1): C.ts("dve", o, x, s1, s2, op0, op1, [rk], [rk])
            gmax = r_[:, 20:21]; goh = r_[:, 24:28]; ngmax = r_[:, 21:22]; gsum = r_[:, 22:23]; gw = r_[:, 23:24]
            C.red(gmax, r_[:, 0:4], ALU.max, [rk], [rk])
            RS(goh, r_[:, 0:4], gmax, None, ALU.is_equal, None)
            RS(ngmax, gmax, -1.0, None, ALU.mult, None)
            C.act(r_[:, 28:32], r_[:, 0:4], AF.Exp, [rk], [rk], bias=ngmax, accum=gsum)
            C.recip(gw, gsum, [rk], [rk])
            prod = r_[:, 32:48]
            R(prod.rearrange("p (g j) -> p g j", j=4), r_[:, 4:20].rearrange("p (g j) -> p g j", j=4),
              goh.unsqueeze(2).to_broadcast([128, 4, 4]), ALU.mult)
            ein = r_[:, 48:52]
            C.red(ein, prod.rearrange("p (g j) -> p j g", j=4), ALU.add, [rk], [rk])
            m1 = r_[:, 52:53]; oh1 = r_[:, 56:60]; e2 = r_[:, 60:64]; m2 = r_[:, 53:54]; oh2 = r_[:, 64:68]
            C.red(m1, ein, ALU.max, [rk], [rk])
            RS(oh1, ein, m1, None, ALU.is_equal, None)
            C.stt(e2, oh1, -1e30, ein, ALU.mult, ALU.add, [rk], [rk])
            C.red(m2, e2, ALU.max, [rk], [rk])
            RS(oh2, e2, m2, None, ALU.is_equal, None)
            dm = r_[:, 54:55]; ed = r_[:, 55:56]; w1 = r_[:, 68:69]; w2 = r_[:, 69:70]; dn = r_[:, 70:71]
            R(dm, m2, m1, ALU.subtract)
            C.act(ed, dm, AF.Exp, [rk], [rk])
            RS(dn, ed, 1.0, None, ALU.add, None)
            C.recip(dn, dn, [rk], [rk])
            R(w1, dn, gw, ALU.mult)
            R(w2, ed, w1, ALU.mult)
            c4 = r_[:, 72:76]
            RS(c4, oh1, w1, None, ALU.mult, None)
            C.stt(c4, oh2, w2, c4, ALU.mult, ALU.add, [rk], [rk])
            C.tt("dve", cw[:, tt, :].rearrange("p (g j) -> p g j", j=4), goh.unsqueeze(2).to_broadcast([128, 4, 4]),
                 c4.unsqueeze(1).to_broadcast([128, 4, 4]), ALU.mult, [rk], ["cw"])
        C.dma("sp", CW[:, :, :], cw[:, :, :], "cwout", ["cw"], ["CW"])
        C.dma("sp", HT2[:, :, :], hT[:, :, :], "ht2out", [("hT", t_) for t_ in range(NT)], ["HT2"])
        sb.release(m0)

    def phase_moe(l):
        m0 = sb.mark()
        gB = sb.alloc("gateB", [128, 2, 2048], F32)
        build_gateB(gB, 5)
        cw = sb.alloc("cw", [128, NT, 16], F32)
        C.dma("sp", cw[:, :, :], CW[:, :, :], "cwin", ["CW"], ["cw"])
        HALF = 9 * 128
        hTh = sb.alloc("hTh", [128, 16, HALF], BF16)
        acc = sb.alloc("moeacc", [128, 9, 2048], F32)
        w1b = [sb.alloc(f"w1b{i}", [128, 16, 256], BF16) for i in range(2)]
        w3b = [sb.alloc(f"w3b{i}", [128, 16, 256], BF16) for i in range(2)]
        w2b = [sb.alloc(f"w2b{i}", [128, 2, 2048], BF16) for i in range(2)]
        actT = [sb.alloc(f"actT{i}", [128, 2, HALF], BF16) for i in range(2)]
        sil = [sb.alloc(f"sil{i}", [128, 512], F32) for i in range(2)]
        evs = [sb.alloc(f"rev{i}", [128, 512], F32) for i in range(2)]
        hch = [(0, 512), (512, 512), (1024, 128)]
        it = 0; ucnt = 0; dcnt = 0; rcnt = 0
        for half in range(2):
            C.dma("sp", hTh[:, :, :], HT2[:, :, half * HALF:(half + 1) * HALF], "hTh", ["HT2"], ["hTh"])
            for e_i in range(16):
                for q in range(4):
                    b = it % 2; it += 1
                    W1, W3, W2, A_ = w1b[b], w3b[b], w2b[b], actT[b]
                    k1, k3, k2, ka = f"w1b{b}", f"w3b{b}", f"w2b{b}", f"actT{b}"
                    p.dma("pool", [lambda e, W1=W1, e_i=e_i, q=q: e.dma_start(
                        out=W1[:, :, :], in_=I["moe_w1"][l, e_i].rearrange("(k p) n -> p k n", p=128)[:, :, q * 256:(q + 1) * 256])], k1, [], [k1])
                    p.dma("pool", [lambda e, W3=W3, e_i=e_i, q=q: e.dma_start(
                        out=W3[:, :, :], in_=I["moe_w3"][l, e_i].rearrange("(k p) n -> p k n", p=128)[:, :, q * 256:(q + 1) * 256])], k3, [], [k3])
                    p.dma("pool", [lambda e, W2=W2, e_i=e_i, q=q: e.dma_start(
                        out=W2[:, :, :], in_=I["moe_w2"][l, e_i, q * 256:(q + 1) * 256, :].rearrange("(k p) n -> p k n", p=128))], k2, [], [k2])
                    for (t0, tw) in hch:
                        for f in range(2):
                            u = ucnt % 2; ucnt += 1
                            for kc in range(16):
                                C.mm(PS[u][:, 0:tw], W1[:, kc, f * 128:(f + 1) * 128], hTh[:, kc, t0:t0 + tw], kc == 0, kc == 15, [k1, "hTh"], [f"ps{u}"])
                            for kc in range(16):
                                C.mm(PS[2 + u][:, 0:tw], W3[:, kc, f * 128:(f + 1) * 128], hTh[:, kc, t0:t0 + tw], kc == 0, kc == 15, [k3, "hTh"], [f"ps{2 + u}"])
                            sl_ = sil[u]; sk_ = f"sil{u}"
                            C.act(sl_[:, 0:tw], PS[u][:, 0:tw], AF.Silu, [f"ps{u}"], [sk_])
                            C.tt("dve", A_[:, f, t0:t0 + tw], PS[2 + u][:, 0:tw], sl_[:, 0:tw], ALU.mult, [f"ps{2 + u}", sk_], [ka])
                    first = (e_i == 0 and q == 0)
                    for ti in range(9):
                        tg = half * 9 + ti
                        for nb in range(4):
                            pb = 4 + dcnt % 4; dcnt += 1
                            for f in range(2):
                                C.mm(PS[pb][:, :], A_[:, f, ti * 128:(ti + 1) * 128], W2[:, f, nb * 512:(nb + 1) * 512], f == 0, f == 1, [ka, k2], [f"ps{pb}"])
                            dst = acc[:, ti, nb * 512:(nb + 1) * 512]
                            if first:
                                C.ts("dve", dst, PS[pb][:, :], cw[:, tg, e_i:e_i + 1], None, ALU.mult, None, [f"ps{pb}", "cw"], [("moeacc", ti, nb)])
                            else:
                                C.stt(dst, PS[pb][:, :], cw[:, tg, e_i:e_i + 1], dst, ALU.mult, ALU.add, [f"ps{pb}", "cw", ("moeacc", ti, nb)], [("moeacc", ti, nb)])
            for ti in range(9):
                tg = half * 9 + ti
                for nb in range(4):
                    resid_update(acc[:, ti, nb * 512:(nb + 1) * 512], gB, tg, nb, [("moeacc", ti, nb)], evs, rcnt)
                    rcnt += 1
        sb.release(m0)

    for l in range(n_layers):
        if stop_after == "init":
            break
        phase_mod(l)
        if stop_after == "mod":
            break
        p.fence()
        m1 = sb.mark()
        hT = sb.alloc("hT", [128, 16, T], BF16)
        phase_norm(l, 0, hT)
        if "HT" in dbg:
            HT = dram_tmp("HT", [128, 16, T], BF16)
            C.dma("sp", HT[:, :, :], hT[:, :, :], "dbg", [("hT", t_) for t_ in range(NT)], ["HT"])
        if stop_after == "norm":
            break
        phase_inproj(l, hT)
        sb.release(m1)
        p.fence()
        if stop_after == "inproj":
            break
        lam_init = 0.8 - 0.6 * math.exp(-0.3 * l)
        if only is None or "s5" in only:
            phase_s5(l)
        if only is None or "win" in only:
            phase_win(l)
        if only is None or "diff" in only:
            phase_diff(l, lam_init)
        if only is None or "na" in only:
            phase_na(l)
        if stop_after == "attn":
            break
        phase_merge(l)
        if stop_after == "merge":
            break
        phase_wout(l)
        if stop_after == "wout":
            break
        m2 = sb.mark()
        hT = sb.alloc("hT", [128, 16, T], BF16)
        phase_router_norm(l, hT)
        sb.release(m2)
        if stop_after == "norm2":
            break
        if only is None or "moe" in only:
            phase_moe(l)

    fin = []
    if stop_after is None:
        p.fence()
        m_f = sb.mark()
        cpf = [sb.alloc(f"cpf{i}", [128, D], F32) for i in range(2)]
        for tt in range(NCT, NT):
            i = tt % 2
            C.dma("sp", cpf[i][:, :], XR[tt * 128:(tt + 1) * 128, :], f"cpb{i}", [("XR", tt)], [f"cpf{i}"])
            C.dma("sp", out[(tt - NCT) * 128:(tt - NCT + 1) * 128, :], cpf[i][:, :], "outw", [f"cpf{i}"], [("out", tt)])
        sb.release(m_f)
        fin.append("outw")
    else:
        C.dma("sp", out[0:128, 0:128], ident[:, :], "outw", ["ident"], [("out", 0)])
        fin.append("outw")
    if "dbg" in p.dsem:
        fin.append("dbg")
    for k in ("pev0", "pev1", "sgv0", "sgv1"):
        if k in p.dsem:
            fin.append(k)
    p.emit(final_sems=fin)
    return nc


SEQ_ = 2048


def prep_inputs(inp, b, nl=DEPTH):
    m = {}
    m["x"] = np.ascontiguousarray(inp["x"][b])
    m["ctx"] = np.ascontiguousarray(inp["ctx"][b])
    cT = np.stack([inp["c"][b].reshape(16, 128).T, inp["c_ctx"].reshape(16, 128).T], axis=-1)
    m["cT"] = np.ascontiguousarray(cT.astype(np.float32))
    m["w_ada"] = inp["w_ada"][:nl]
    m["b_adaT"] = np.ascontiguousarray(inp["b_ada"].reshape(-1, 96, 128).transpose(0, 2, 1)[:nl])
    nm = np.stack([inp["norm_mix"], inp["norm_ffn"]], axis=1)
    m["normT"] = np.ascontiguousarray(nm.reshape(-1, 2, 16, 128).transpose(0, 1, 3, 2)[:nl])
    m["w_in"] = inp["w_in"][:nl]
    m.update(host_consts())
    m["qkg"] = np.ascontiguousarray(np.concatenate([inp[k][:nl] for k in ("win_qn", "win_kn", "diff_qn", "diff_kn", "na_qn", "na_kn")], axis=1))
    m["sink"] = np.ascontiguousarray(inp["win_sink"][:nl])
    m["dlam"] = np.ascontiguousarray(inp["diff_lambda"][:nl].reshape(nl, 256))
    m["subg"] = np.ascontiguousarray(inp["diff_subln"][:nl])
    m["nab"] = na_bias_layout(inp["na_rpb"][:nl])
    def dp(a):
        a = np.moveaxis(a[:nl], 3, 2)
        return a.reshape((nl, 128) + a.shape[3:])
    m["s5a"] = np.ascontiguousarray(np.stack([dp(inp["s5_a_re"]), dp(inp["s5_a_im"])], axis=-1))
    m["s5ls"] = np.ascontiguousarray(np.repeat(inp["s5_log_step"][:nl][:, :, None, :], 64, axis=2).reshape(nl, 128, 32))
    m["s5b"] = np.ascontiguousarray(np.stack([dp(inp["s5_b_re"]), dp(inp["s5_b_im"])], axis=3))
    cre = np.swapaxes(inp["s5_c_re"], 3, 4); cim = np.swapaxes(inp["s5_c_im"], 3, 4)
    m["s5c"] = np.ascontiguousarray(np.stack([dp(cre), dp(cim)], axis=3))
    dg = inp["s5_d"][:nl].reshape(nl, 32, 16)
    m["s5d"] = np.ascontiguousarray(np.tile(dg.transpose(0, 2, 1)[:, None, :, :], (1, 8, 1, 1)).reshape(nl, 128, 32))
    m["s5_w_glu"] = inp["s5_w_glu"][:nl]
    m["s5_b_glu"] = inp["s5_b_glu"][:nl]
    m["w_branch"] = np.ascontiguousarray(inp["w_branch"][:nl].reshape(nl, 2048, 2048))
    m["w_out"] = inp["w_out"][:nl]
    wr = np.concatenate([inp["moe_w_group"][:nl], inp["moe_w_expert"][:nl]], axis=2)
    m["wrT"] = np.ascontiguousarray(wr.reshape(nl, 16, 128, 20).transpose(0, 2, 1, 3).reshape(nl, 128, 320))
    m["brt"] = np.ascontiguousarray(np.concatenate([inp["moe_b_group"][:nl], inp["moe_b_expert"][:nl]], axis=1))
    m["moe_w1"] = inp["moe_w1"][:nl]
    m["moe_w3"] = inp["moe_w3"][:nl]
    m["moe_w2"] = inp["moe_w2"][:nl]
    return m


_CONSTS = {}


def host_consts():
    if not _CONSTS:
        t = np.arange(2048)
        row = (t // 64).astype(np.float32); col = (t % 64).astype(np.float32)
        inv = (100.0 ** (-np.arange(16, dtype=np.float32) / 16)).astype(np.float32)
        ang = np.concatenate([row[:, None] * inv, col[:, None] * inv], axis=-1).astype(np.float32)
        cs = np.stack([np.cos(ang), np.sin(ang)], axis=1).astype(np.float32)
        _CONSTS["rope"] = np.ascontiguousarray(cs.reshape(16, 128, 2, 32).transpose(1, 0, 2, 3))
        j = np.arange(128)[:, None]; i = np.arange(128)[None, :]
        mL = (i <= j).astype(np.float32); mR = (j <= i).astype(np.float32)
        _CONSTS["wmask"] = np.ascontiguousarray(np.stack([np.tile(mL, (1, 4)), np.tile(mR, (1, 4))], axis=0))
        tau = np.arange(128)[:, None] // 16; ii = np.arange(128)[None, :] // 16
        _CONSTS["s5mask"] = np.ascontiguousarray(np.stack([(tau <= ii), (tau >= ii)], axis=0).astype(np.float32))
    return dict(_CONSTS)


def na_bias_layout(rpb):
    nl = rpb.shape[0]
    out = np.full((nl, 5, 128, 8, 5, 128), NEG, np.float32)
    rep_n = [0, 1, 5, 14, 15]
    qi = np.arange(128); qr_l = qi // 64; qc = qi % 64
    ki = np.arange(128); kr_l = ki // 64; kc = ki % 64
    cstart = np.clip(qc - 8, 0, 48)
    col_ok = (kc[None, :] >= cstart[:, None]) & (kc[None, :] < cstart[:, None] + 16)
    c_off = np.clip(kc[None, :] - qc[:, None] + 15, 0, 30)
    for ty, n in enumerate(rep_n):
        r0 = 2 * n
        s0 = min(max(r0 - 4, 0), 24); s1 = min(max(r0 + 1 - 4, 0), 24)
        first = s0 // 2; last = (s1 + 7) // 2
        for sl, kt in enumerate(range(first, last + 1)):
            qrow = r0 + qr_l
            krow = 2 * kt + kr_l
            srow = np.clip(qrow - 4, 0, 24)
            row_ok = (krow[None, :] >= srow[:, None]) & (krow[None, :] < srow[:, None] + 8)
            ok = row_ok & col_ok
            r_off = np.clip(krow[None, :] - qrow[:, None] + 7, 0, 14)
            g = rpb[:, :, r_off, c_off]
            g = np.where(ok[None, None], g, np.float32(NEG))
            out[:, ty, :, :, sl, :] = g.transpose(0, 2, 1, 3)
    return np.ascontiguousarray(out.reshape(nl, 5, 128, 8 * 5 * 128))


_NC_CACHE = {}


def kernel(**inputs):
    inp = {k: np.asarray(v) for k, v in inputs.items()}
    if "full" not in _NC_CACHE:
        _NC_CACHE["full"] = build_program()
    nc = _NC_CACHE["full"]
    in_maps = [prep_inputs(inp, b) for b in range(8)]
    res = run_bass_kernel_spmd(nc, in_maps, core_ids=list(range(8)))
    return np.stack([np.asarray(r["out"]) for r in res.results], axis=0).astype(np.float32)
```

```python
import contextlib
import math
import numpy as np
import ml_dtypes
import concourse.bass as bass
import concourse.mybir as mybir
from concourse.bass_utils import run_bass_kernel_spmd

F32 = mybir.dt.float32
BF16 = mybir.dt.bfloat16
ALU = mybir.AluOpType
AF = mybir.ActivationFunctionType
AX = mybir.AxisListType

D = 2048
T = 2304
NT = 18
NCT = 2
DEPTH = 4
INW = 12544
NPROJ = 4352
EPS = 1e-6
NEG = -30000.0

ENGS = ("pe", "act", "dve", "pool", "sp")
SEM_ROT = 30000


class Prog:
    def __init__(self, nc):
        self.nc = nc
        self.ops = {e: [] for e in ENGS}
        self.lastw = {}
        self.readers = {}
        self.dsem = {}
        self.pending_fence = {e: set() for e in ENGS}

    def _deps(self, r, w):
        deps = set()
        for k in r:
            if k in self.lastw:
                deps.add(self.lastw[k])
        for k in w:
            if k in self.lastw:
                deps.add(self.lastw[k])
            for x in self.readers.get(k, ()):
                deps.add(x)
        return deps

    def _commit(self, me, r, w):
        for k in w:
            self.lastw[k] = me
            self.readers[k] = []
        for k in r:
            if k in w:
                continue
            self.readers.setdefault(k, []).append(me)

    def fence(self):
        deps = set()
        for e in ENGS:
            for i in range(len(self.ops[e]) - 1, -1, -1):
                if self.ops[e][i]["kind"] == "c":
                    deps.add(("e", e, i))
                    break
        for s, c in self.dsem.items():
            deps.add(("d", s, c))
        for e in ENGS:
            self.pending_fence[e] |= deps

    def op(self, eng, fn, r=(), w=(), nosame=False):
        r = tuple(r); w = tuple(w)
        deps = self._deps(r, w)
        if eng == "pe" or nosame:
            deps = {d for d in deps if not (d[0] == "e" and d[1] == eng)}
        deps |= self.pending_fence[eng]
        self.pending_fence[eng] = set()
        idx = len(self.ops[eng])
        me = ("e", eng, idx)
        deps.discard(me)
        self.ops[eng].append(dict(kind="c", fn=fn, deps=deps, sig=False))
        self._commit(me, r, w)
        return me

    def dma(self, q, fns, sem, r=(), w=()):
        r = tuple(r); w = tuple(w)
        deps = self._deps(r, w)
        deps |= self.pending_fence[q]
        self.pending_fence[q] = set()
        cnt = self.dsem.get(sem, 0) + 16 * len(fns)
        self.dsem[sem] = cnt
        me = ("d", sem, cnt)
        self.ops[q].append(dict(kind="d", fns=fns, deps=deps, sem=sem))
        self._commit(me, r, w)
        return me

    def emit(self, final_sems=()):
        nc = self.nc
        for e in ENGS:
            for o in self.ops[e]:
                for d in o["deps"]:
                    if d[0] == "e":
                        self.ops[d[1]][d[2]]["sig"] = True
        signo = {}
        nsig = {}
        for e in ENGS:
            n = 0
            for i, o in enumerate(self.ops[e]):
                if o["kind"] == "c" and o["sig"]:
                    n += 1
                    signo[(e, i)] = n
            nsig[e] = n
        with contextlib.ExitStack() as st:
            esems = {}
            for e in ENGS:
                k = max(1, (nsig[e] + SEM_ROT - 1) // SEM_ROT)
                esems[e] = [st.enter_context(nc.semaphore(f"s_{e}{j}")) for j in range(k)]
            dsems = {name: st.enter_context(nc.semaphore(f"d_{name}")) for name in self.dsem}
            self.nsems = sum(len(v) for v in esems.values()) + len(dsems)

            def semval(d):
                if d[0] == "e":
                    n = signo[(d[1], d[2])]
                    j = (n - 1) // SEM_ROT
                    return (("e", d[1], j), esems[d[1]][j], n - j * SEM_ROT)
                return (("d", d[1]), dsems[d[1]], d[2])

            block = st.enter_context(nc.Block())

            def run_queue(e, eng):
                seen = {}
                for i, o in enumerate(self.ops[e]):
                    need = {}
                    for d in o["deps"]:
                        key, sem, val = semval(d)
                        if seen.get(key, 0) >= val:
                            continue
                        if need.get(key, (None, 0))[1] < val:
                            need[key] = (sem, val)
                    for key, (sem, val) in need.items():
                        eng.wait_ge(sem, val)
                        seen[key] = val
                    if o["kind"] == "c":
                        ins = o["fn"](eng)
                        if o["sig"]:
                            n = signo[(e, i)]
                            ins.then_inc(esems[e][(n - 1) // SEM_ROT], 1)
                    else:
                        for f in o["fns"]:
                            f(eng).then_inc(dsems[o["sem"]], 16)
                if e == "sp":
                    for name in final_sems:
                        eng.wait_ge(dsems[name], self.dsem[name])

            @block.tensor
            def _(eng):
                run_queue("pe", eng)

            @block.scalar
            def _(eng):
                run_queue("act", eng)

            @block.vector
            def _(eng):
                run_queue("dve", eng)

            @block.gpsimd
            def _(eng):
                run_queue("pool", eng)

            @block.sync
            def _(eng):
                run_queue("sp", eng)


class SB:
    def __init__(self, nc, base=16512, limit=229312):
        self.nc = nc
        self.off = base
        self.limit = limit
        self.n = 0
        self.prog = None
        self.dirty = False

    def alloc(self, name, shape, dtype):
        if self.dirty:
            self.prog.fence()
            self.dirty = False
        sz = 1
        for s in shape[1:]:
            sz *= s
        nbytes = sz * (2 if dtype == BF16 else 4)
        nbytes = (nbytes + 63) // 64 * 64
        off = self.off
        assert off + nbytes <= self.limit, f"SBUF overflow at {name}: {off}+{nbytes}"
        self.off += nbytes
        self.n += 1
        return self.nc.alloc_sbuf_tensor_at(f"{name}_{self.n}", list(shape), dtype, offset=off)

    def mark(self):
        return self.off

    def release(self, m):
        self.off = m
        self.dirty = True


class Ctx:
    pass


def _mk(C):
    p = C.p

    def mm(out, lhsT, rhs, start, stop, r, w):
        p.op("pe", lambda e: e.matmul(out, lhsT=lhsT, rhs=rhs, start=start, stop=stop), r, w)

    C.bank_open = {}

    def amm(bank, out, lhsT, rhs, r, w):
        first = not C.bank_open.get(bank, False)
        C.bank_open[bank] = True
        p.op("pe", lambda e: e.matmul(out, lhsT=lhsT, rhs=rhs, start=first, stop=False, skip_group_check=True), r, w)

    def adone(bank):
        C.bank_open[bank] = False

    C.amm, C.adone = amm, adone

    def tr(out, in_, ident, r, w):
        p.op("pe", lambda e: e.transpose(out=out, in_=in_, identity=ident), r, w)

    def act(out, in_, func, r, w, scale=1.0, bias=None, accum=None, nosame=False):
        def f(e):
            kw = {}
            if bias is not None:
                kw["bias"] = bias
            if accum is not None:
                kw["accum_out"] = accum
            return e.activation(out=out, in_=in_, func=func, scale=scale, **kw)
        p.op("act", f, r, w, nosame=nosame)

    def tt(eng, out, a, b, op, r, w):
        p.op(eng, lambda e: e.tensor_tensor(out=out, in0=a, in1=b, op=op), r, w)

    def ts(eng, out, a, s1, s2, op0, op1, r, w):
        if op1 is None:
            p.op(eng, lambda e: e.tensor_scalar(out=out, in0=a, scalar1=s1, scalar2=None, op0=op0), r, w)
        else:
            p.op(eng, lambda e: e.tensor_scalar(out=out, in0=a, scalar1=s1, scalar2=s2, op0=op0, op1=op1), r, w)

    def stt(out, a, s, b, op0, op1, r, w):
        p.op("dve", lambda e: e.scalar_tensor_tensor(out=out, in0=a, scalar=s, in1=b, op0=op0, op1=op1), r, w)

    def cp(eng, out, in_, r, w):
        if eng == "act":
            p.op("act", lambda e: e.activation(out=out, in_=in_, func=AF.Copy), r, w)
        else:
            p.op(eng, lambda e: e.tensor_copy(out=out, in_=in_), r, w)

    def red(out, in_, op, r, w, axis=AX.X):
        p.op("dve", lambda e: e.tensor_reduce(out=out, in_=in_, axis=axis, op=op), r, w)

    def recip(out, in_, r, w):
        p.op("dve", lambda e: e.reciprocal(out=out, in_=in_), r, w)

    def memset(eng, ap, val, w):
        p.op(eng, lambda e: e.memset(ap, val), (), w)

    def dma(q, out, in_, sem, r, w):
        p.dma(q, [lambda e: e.dma_start(out=out, in_=in_)], sem, r, w)

    C.mm, C.tr, C.act, C.tt, C.ts, C.stt, C.cp, C.red, C.recip, C.memset, C.dma = \
        mm, tr, act, tt, ts, stt, cp, red, recip, memset, dma


def tok_lc(tt):
    return 1 if tt < NCT else 0


def build_program(n_layers=DEPTH, stop_after=None, dbg=(), only=None, skip_front=False):
    nc = bass.Bass("TRN2", target_bir_lowering=False)
    C = Ctx()
    C.nc = nc
    C.p = Prog(nc)
    _mk(C)
    p = C.p

    def dram_in(name, shape, dtype=F32):
        return nc.dram_tensor(name, list(shape), dtype, kind="ExternalInput").ap()

    def dram_tmp(name, shape, dtype=F32):
        kind = "ExternalOutput" if name in dbg else "Internal"
        return nc.dram_tensor(name, list(shape), dtype, kind=kind).ap()

    I = {}
    C.I = I
    I["x"] = dram_in("x", [2048, D])
    I["ctx"] = dram_in("ctx", [256, D])
    I["cT"] = dram_in("cT", [128, 16, 2])
    I["w_ada"] = dram_in("w_ada", [n_layers, D, 6 * D])
    I["b_adaT"] = dram_in("b_adaT", [n_layers, 128, 96])
    I["normT"] = dram_in("normT", [n_layers, 2, 128, 16])
    I["w_in"] = dram_in("w_in", [n_layers, D, INW])
    I["rope"] = dram_in("rope", [128, 16, 2, 32])
    I["wmask"] = dram_in("wmask", [2, 128, 512])
    I["qkg"] = dram_in("qkg", [n_layers, 6 * 64])
    I["sink"] = dram_in("sink", [n_layers, 8])
    I["dlam"] = dram_in("dlam", [n_layers, 4 * 64])
    I["subg"] = dram_in("subg", [n_layers, 128])
    I["nab"] = dram_in("nab", [n_layers, 5, 128, 8 * 5 * 128])
    I["s5a"] = dram_in("s5a", [n_layers, 128, 32, 2])
    I["s5ls"] = dram_in("s5ls", [n_layers, 128, 32])
    I["s5b"] = dram_in("s5b", [n_layers, 128, 32, 2, 16])
    I["s5c"] = dram_in("s5c", [n_layers, 128, 32, 2, 16])
    I["s5d"] = dram_in("s5d", [n_layers, 128, 32])
    I["s5mask"] = dram_in("s5mask", [2, 128, 128])
    I["s5_w_glu"] = dram_in("s5_w_glu", [n_layers, 512, 512])
    I["s5_b_glu"] = dram_in("s5_b_glu", [n_layers, 512])
    I["w_branch"] = dram_in("w_branch", [n_layers, 2048, 2048])
    I["w_out"] = dram_in("w_out", [n_layers, 2048, 2048])
    I["wrT"] = dram_in("wrT", [n_layers, 128, 16 * 20])
    I["brt"] = dram_in("brt", [n_layers, 20])
    I["moe_w1"] = dram_in("moe_w1", [n_layers, 16, 2048, 1024])
    I["moe_w3"] = dram_in("moe_w3", [n_layers, 16, 2048, 1024])
    I["moe_w2"] = dram_in("moe_w2", [n_layers, 16, 1024, 2048])
    out = nc.dram_tensor("out", [2048, D], F32, kind="ExternalOutput").ap()

    XR = dram_tmp("XR", [T, D])
    PROJ = dram_tmp("PROJ", [T, NPROJ])
    SG = dram_tmp("SG", [64, 128, T], BF16)
    MODD = dram_tmp("MODD", [128, 96, 2])
    YT = dram_tmp("YT", [16, 128, T], BF16)
    YS = dram_tmp("YS", [T, 512])
    MG = dram_tmp("MG", [16, 128, T], BF16)
    HT2 = dram_tmp("HT2", [128, 16, T], BF16)
    CW = dram_tmp("CW", [128, NT, 16])

    sb = SB(nc)
    sb.prog = p
    PS = [nc.alloc_psum_tensor(f"ps{i}", [128, 512], F32) for i in range(8)]
    C.PS = PS
    ident = sb.alloc("ident", [128, 128], F32)
    identb = sb.alloc("identb", [128, 128], BF16)
    C.memset("pool", ident[:, :], 0.0, ["ident"])
    p.op("pool", lambda e: e.affine_select(out=ident[:, :], in_=ident[:, :], pattern=[[-1, 128]], compare_op=ALU.not_equal,
                                           fill=1.0, base=0, channel_multiplier=1), ["ident"], ["ident"])
    C.cp("pool", identb[:, :], ident[:, :], ["ident"], ["identb"])
    ones = sb.alloc("ones", [128, 128], F32)
    C.memset("pool", ones[:, :], 1.0, ["ones"])
    scT = sb.alloc("scT", [128, 16, 2], F32)
    C.dma("sp", scT[:, :, :], I["cT"][:, :, :], "scT", [], ["scT"])
    C.act(scT[:, :, :], scT[:, :, :], AF.Silu, ["scT"], ["scT"])
    modT = sb.alloc("modT", [128, 96, 2], F32)
    Amod = sb.alloc("Amod", [128, 2, 16, 2], F32)
    normg = sb.alloc("normg", [128, 2, 16], F32)
    bada = sb.alloc("bada", [128, 96], F32)
    gmark = sb.mark()

    m_init = sb.mark()
    cpb = [sb.alloc(f"cpb{i}", [128, D], F32) for i in range(2)]
    for tt in range(NT):
        i = tt % 2
        src = I["ctx"][tt * 128:(tt + 1) * 128, :] if tt < NCT else I["x"][(tt - NCT) * 128:(tt - NCT + 1) * 128, :]
        C.dma("sp", cpb[i][:, :], src, f"cpb{i}", [], [f"cpb{i}"])
        C.dma("sp", XR[tt * 128:(tt + 1) * 128, :], cpb[i][:, :], f"cpo{i}", [f"cpb{i}"], [("XR", tt)])
    sb.release(m_init)
    p.fence()

    def phase_mod(l):
        m0 = sb.mark()
        wb = [sb.alloc(f"wada{i}", [128, 16, 256], F32) for i in range(2)]
        C.dma("sp", bada[:, :], I["b_adaT"][l, :, :], "bada", [], ["bada"])
        C.dma("sp", normg[:, :, :], I["normT"][l].rearrange("s p k -> p s k"), "normg", [], ["normg"])
        pm = PS[7][:, 0:192].rearrange("p (m c) -> p m c", c=2)
        wsrc = I["w_ada"][l].rearrange("(kc p) n -> p kc n", p=128)
        for blk in range(48):
            b = wb[blk % 2]
            C.dma("sp", b[:, :, :], wsrc[:, :, blk * 256:(blk + 1) * 256], f"wada{blk % 2}", [], [f"wada{blk % 2}"])
            for mi in range(2):
                m = blk * 2 + mi
                for kc in range(16):
                    C.mm(pm[:, m, :], b[:, kc, mi * 128:(mi + 1) * 128], scT[:, kc, :], kc == 0, kc == 15,
                         [f"wada{blk % 2}", "scT"], ["ps7"])
        C.tt("dve", modT[:, :, :], pm, bada[:, :].unsqueeze(2).to_broadcast([128, 96, 2]), ALU.add, ["ps7", "bada"], ["modT"])
        for s in range(2):
            j = 1 + 3 * s
            C.ts("dve", Amod[:, s, :, :], modT[:, j * 16:(j + 1) * 16, :], 1.0, None, ALU.add, None, ["modT"], ["Amod"])
            C.tt("dve", Amod[:, s, :, :], Amod[:, s, :, :], normg[:, s, :].unsqueeze(2).to_broadcast([128, 16, 2]), ALU.mult,
                 ["Amod", "normg"], ["Amod"])
        if "MODD" in dbg:
            C.dma("sp", MODD[:, :, :], modT[:, :, :], "dbg", ["modT"], ["MODD"])
        sb.release(m0)

    def phase_norm(l, s, hT, router=None):
        m0 = sb.mark()
        xt = [sb.alloc(f"xt{i}", [128, D], F32) for i in range(2)]
        junk = sb.alloc("junk", [128, D], BF16)
        st = [sb.alloc(f"nst{i}", [128, 4], F32) for i in range(2)]
        import os
        for tt in (range(NT) if not os.environ.get("REVN") else reversed(range(NT))):
            i = tt % 2
            lc = tok_lc(tt)
            x_, s_ = xt[i], st[i]
            C.dma("sp", x_[:, :], XR[tt * 128:(tt + 1) * 128, :], f"xt{i}", [("XR", tt)], [f"xt{i}"])
            C.act(junk[:, :], x_[:, :], AF.Square, [f"xt{i}"], ["junk", f"nst{i}a"], accum=s_[:, 0:1])
            C.ts("dve", s_[:, 1:2], s_[:, 0:1], 1.0 / D, EPS, ALU.mult, ALU.add, [f"nst{i}a"], [f"nst{i}b"])
            C.act(s_[:, 2:3], s_[:, 1:2], AF.Sqrt, [f"nst{i}b"], [f"nst{i}c"])
            C.recip(s_[:, 3:4], s_[:, 2:3], [f"nst{i}c"], [f"nst{i}d"])
            C.ts("dve", x_[:, :], x_[:, :], s_[:, 3:4], None, ALU.mult, None, [f"xt{i}", f"nst{i}d"], [f"xt{i}"])
            for g4 in range(4):
                pb = 2 + (g4 % 2)
                pT = PS[pb][:, :].rearrange("p (a b) -> p a b", b=128)
                for a in range(4):
                    kc = g4 * 4 + a
                    C.tr(pT[:, a, :], x_[:, kc * 128:(kc + 1) * 128], ident[:, :], [f"xt{i}", "ident"], [f"ps{pb}"])
                for a in range(4):
                    kc = g4 * 4 + a
                    C.act(hT[:, kc, tt * 128:(tt + 1) * 128], pT[:, a, :], AF.Identity, [f"ps{pb}", "Amod", "modT"],
                          [("hT", tt)], scale=Amod[:, s, kc, lc:lc + 1], bias=modT[:, (3 * s) * 16 + kc, lc:lc + 1], nosame=True)
        sb.release(m0)

    def phase_inproj(l, hT):
        m0 = sb.mark()
        wb = [sb.alloc(f"win{i}", [128, 16, 512], BF16) for i in range(2)]
        ev = [sb.alloc(f"pev{i}", [128, 512], F32) for i in range(2)]
        sg = [sb.alloc(f"sgv{i}", [128, 512], BF16) for i in range(2)]
        wsrc = I["w_in"][l].rearrange("(kc p) n -> p kc n", p=128)
        nblk = 0
        cnt = 0
        for c0 in range(0, NPROJ, 512):
            cw = min(512, NPROJ - c0)
            b = wb[nblk % 2]; bk = f"win{nblk % 2}"
            C.dma("pool", b[:, :, 0:cw], wsrc[:, :, c0:c0 + cw], bk, [], [bk])
            nblk += 1
            for tt in range(NT):
                pb = cnt % 2
                for kc in range(16):
                    C.mm(PS[pb][:, 0:cw], hT[:, kc, tt * 128:(tt + 1) * 128], b[:, kc, 0:cw], kc == 0, kc == 15,
                         [("hT", tt), bk], [f"ps{pb}"])
                e_ = ev[cnt % 2]; ek = f"pev{cnt % 2}"
                C.cp("dve" if cnt % 2 == 0 else "act", e_[:, 0:cw], PS[pb][:, 0:cw], [f"ps{pb}"], [ek])
                C.dma("sp", PROJ[tt * 128:(tt + 1) * 128, c0:c0 + cw], e_[:, 0:cw], ek, [ek], [("PROJ", tt, c0 // 512)])
                cnt += 1
        tchunks = [(0, 512), (512, 512), (1024, 512), (1536, 512), (2048, 256)]
        for g4 in range(16):
            c0 = NPROJ + g4 * 512
            b = wb[nblk % 2]; bk = f"win{nblk % 2}"
            C.dma("pool", b[:, :, :], wsrc[:, :, c0:c0 + 512], bk, [], [bk])
            nblk += 1
            for a in range(4):
                gi = g4 * 4 + a
                for (t0, tw) in tchunks:
                    pb = cnt % 2
                    for kc in range(16):
                        C.mm(PS[pb][:, 0:tw], b[:, kc, a * 128:(a + 1) * 128], hT[:, kc, t0:t0 + tw], kc == 0, kc == 15,
                             [("hT", t_) for t_ in range(NT)] + [bk], [f"ps{pb}"])
                    s_ = sg[cnt % 2]; sk = f"sgv{cnt % 2}"
                    C.act(s_[:, 0:tw], PS[pb][:, 0:tw], AF.Sigmoid, [f"ps{pb}"], [sk])
                    C.dma("sp", SG[gi, :, t0:t0 + tw], s_[:, 0:tw], sk, [sk], [("SG", gi)])
                    cnt += 1
        sb.release(m0)


    def bcast_load(dst, src_row, n, sem, key):
        C.dma("sp", dst, src_row.partition_broadcast(128), sem, [], [key])

    def prep_qk(l, col0, nh, gidx, rope, dst, qscale=None, dup=False, tag="q"):
        m0 = sb.mark()
        W = nh * 64
        src = [sb.alloc(f"pq_src{i}", [128, W], F32) for i in range(2)]
        tmp = [sb.alloc(f"pq_tmp{i}", [128, W], F32) for i in range(2)]
        o = [sb.alloc(f"pq_o{i}", [128, (2 * W if dup else W)], F32) for i in range(2)]
        stt_ = [sb.alloc(f"pq_st{i}", [128, 4, 8], F32) for i in range(2)]
        gB = sb.alloc("pq_g", [128, 64], F32)
        bcast_load(gB[:, :], I["qkg"][l, gidx * 64:(gidx + 1) * 64], 64, "pq_g", "pq_g")
        if qscale is not None:
            C.ts("dve", gB[:, :], gB[:, :], qscale, None, ALU.mult, None, ["pq_g"], ["pq_g"])
        cs = sb.alloc("pq_cs", [128, 16, 2, 32], F32)
        if rope:
            C.dma("sp", cs[:, :, :, :], I["rope"][:, :, :, :], "pq_cs", [], ["pq_cs"])
        for tt in range(NT):
            i = tt % 2
            S_, T_, O_, st_ = src[i], tmp[i], o[i], stt_[i]
            ks, kt_, ko, kst = f"pq_src{i}", f"pq_tmp{i}", f"pq_o{i}", f"pq_st{i}"
            C.dma("sp", S_[:, :], PROJ[tt * 128:(tt + 1) * 128, col0:col0 + W], ks, [("PROJ", tt, c) for c in range(9)], [ks])
            S3 = S_[:, :].rearrange("p (h d) -> p h d", d=64)
            T3 = T_[:, :].rearrange("p (h d) -> p h d", d=64)
            C.tt("dve", T_[:, :], S_[:, :], S_[:, :], ALU.mult, [ks], [kt_])
            C.red(st_[:, 0, 0:nh], T3, ALU.add, [kt_], [kst])
            C.ts("dve", st_[:, 1, 0:nh], st_[:, 0, 0:nh], 1.0 / 64, EPS, ALU.mult, ALU.add, [kst], [kst])
            C.act(st_[:, 2, 0:nh], st_[:, 1, 0:nh], AF.Sqrt, [kst], [kst])
            C.recip(st_[:, 3, 0:nh], st_[:, 2, 0:nh], [kst], [kst])
            C.tt("dve", T3, S3, st_[:, 3, 0:nh].unsqueeze(2).to_broadcast([128, nh, 64]), ALU.mult, [ks, kst], [kt_])
            C.tt("dve", T3, T3, gB[:, :].unsqueeze(1).to_broadcast([128, nh, 64]), ALU.mult, [kt_, "pq_g"], [kt_])
            if dup:
                O4 = O_[:, :].rearrange("p (h u d) -> p h u d", u=2, d=64)
                Of = [O4[:, :, 0, :], O4[:, :, 1, :]]
            else:
                Of = [O_[:, :].rearrange("p (h d) -> p h d", d=64)]
            if rope and tt >= NCT:
                n = tt - NCT
                cosb = cs[:, n, 0, :].unsqueeze(1).to_broadcast([128, nh, 32])
                sinb = cs[:, n, 1, :].unsqueeze(1).to_broadcast([128, nh, 32])
                x1 = T3[:, :, 0:32]; x2 = T3[:, :, 32:64]
                A3 = S3
                C.tt("dve", A3[:, :, 0:32], x1, cosb, ALU.mult, [kt_, "pq_cs"], [ks])
                C.tt("dve", A3[:, :, 32:64], x2, sinb, ALU.mult, [kt_, "pq_cs"], [ks])
                for Oo in Of:
                    C.tt("dve", Oo[:, :, 0:32], A3[:, :, 0:32], A3[:, :, 32:64], ALU.subtract, [ks], [ko])
                C.tt("dve", A3[:, :, 0:32], x1, sinb, ALU.mult, [kt_, "pq_cs", ko], [ks])
                C.tt("dve", A3[:, :, 32:64], x2, cosb, ALU.mult, [kt_, "pq_cs"], [ks])
                for Oo in Of:
                    C.tt("dve", Oo[:, :, 32:64], A3[:, :, 0:32], A3[:, :, 32:64], ALU.add, [ks], [ko])
            else:
                for Oo in Of:
                    C.cp("dve", Oo, T3, [kt_], [ko])
            npair = (2 * W if dup else W) // 128
            for g4 in range(0, npair, 4):
                pb = 2 + ((g4 // 4) % 2)
                na = min(4, npair - g4)
                pT = PS[pb][:, :].rearrange("p (a b) -> p a b", b=128)
                for a in range(na):
                    C.tr(pT[:, a, :], O_[:, (g4 + a) * 128:(g4 + a + 1) * 128], ident[:, :], [ko, "ident"], [f"ps{pb}"])
                C.act(dst[:, g4:g4 + na, tt * 128:(tt + 1) * 128], pT[:, 0:na, :], AF.Copy, [f"ps{pb}"], [(tag, tt)])
        sb.release(m0)

    def load_v(col0, nkv, dv, Vt, key):
        C.memset("dve", Vt[:, :, :, dv:dv + 1], 1.0, [key])
        for t in range(NT):
            p.dma("pool", [lambda e, t=t: e.dma_start(
                out=Vt[:, t, :, 0:dv],
                in_=PROJ[t * 128:(t + 1) * 128, col0:col0 + nkv * dv].rearrange("p (h d) -> p h d", d=dv))],
                key, [("PROJ", t, c) for c in range(9)] + [key], [key])

    def emit_y(ytile, branch, tq, ykey):
        yb = C.yb[tq % 2]; ybk = f"yb{tq % 2}"
        C.cp("dve", yb[:, :], ytile, [ykey], [ybk])
        pTb = PS[6][:, :].bitcast(BF16).rearrange("p (a b) -> p a b", b=128)
        for a in range(4):
            C.tr(pTb[:, a, :], yb[:, a * 128:(a + 1) * 128], identb[:, :], [ybk, "identb"], ["ps6"])
        yt = C.ytb[tq % 2]; ytk = f"ytb{tq % 2}"
        C.cp("act", yt[:, :, :], pTb[:, 0:4, :], ["ps6"], [ytk])
        C.dma("sp", YT[branch * 4:(branch + 1) * 4, :, tq * 128:(tq + 1) * 128].rearrange("k p t -> p k t"), yt[:, :, :],
              ytk, [ytk], [("YT", branch, tq)])

    def alloc_y():
        C.yb = [sb.alloc(f"yb{i}", [128, 512], BF16) for i in range(2)]
        C.ytb = [sb.alloc(f"ytb{i}", [128, 4, 128], BF16) for i in range(2)]

    def phase_win(l):
        m0 = sb.mark()
        qT = sb.alloc("w_qT", [128, 4, T], BF16)
        kT = sb.alloc("w_kT", [128, 2, T], BF16)
        Vt = sb.alloc("w_V", [128, NT, 2, 65], BF16)
        mk = sb.alloc("w_mask", [128, 2, 512], BF16)
        sk = sb.alloc("w_sink", [128, 8], F32)
        p.dma("pool", [lambda e: e.dma_start(out=mk[:, :, :], in_=I["wmask"].rearrange("m p c -> p m c"))], "w_mask", [], ["w_mask"])
        bcast_load(sk[:, :], I["sink"][l, :], 8, "w_sink", "w_sink")
        C.act(sk[:, :], sk[:, :], AF.Exp, ["w_sink"], ["w_sink"])
        load_v(1152, 2, 64, Vt, "w_V")
        prep_qk(l, 512, 8, 0, True, qT, tag="w_q")
        prep_qk(l, 1024, 2, 1, True, kT, dup=True, tag="w_k")
        alloc_y()
        E = [sb.alloc(f"w_E{i}", [128, 512], BF16) for i in range(2)]
        ysb = [sb.alloc(f"w_y{i}", [128, 512], F32) for i in range(2)]
        den = [sb.alloc(f"w_den{i}", [128, 8], F32) for i in range(2)]
        allq = [("w_q", t) for t in range(NT)]; allk = [("w_k", t) for t in range(NT)]
        ecnt = 0
        for tq in range(NT):
            if tq < NCT:
                keys = [(0, None), (1, None)]
            else:
                n = tq - NCT
                keys = [(0, None), (1, None)]
                if n >= 1:
                    keys.append((tq - 1, 0))
                keys.append((tq, None))
                if n <= 14:
                    keys.append((tq + 1, 1))
            y_ = ysb[tq % 2]; yk = f"w_y{tq % 2}"
            d_ = den[tq % 2]; dk = f"w_den{tq % 2}"
            for kh in range(2):
                pO = PS[4 + kh][:, 0:260].rearrange("p (j c) -> p j c", c=65)
                pok = f"ps{4 + kh}"
                for ki, (kt, msk) in enumerate(keys):
                    sbk = ecnt % 2
                    bXY = [PS[2 * sbk], PS[2 * sbk + 1]]
                    for j in range(4):
                        h = kh * 4 + j; half = h % 2; hp = h // 2
                        C.mm(bXY[half][:, (j // 2) * 128:(j // 2 + 1) * 128], kT[half * 64:(half + 1) * 64, kh, kt * 128:(kt + 1) * 128],
                             qT[half * 64:(half + 1) * 64, hp, tq * 128:(tq + 1) * 128], True, True, allq + allk, [f"ps{2 * sbk + half}"])
                    E_ = E[sbk]; ek = f"w_E{sbk}"
                    Ev = E_[:, :].rearrange("p (jj two q) -> p two jj q", two=2, q=128)
                    for half in range(2):
                        C.act(Ev[:, half, :, :], bXY[half][:, 0:256].rearrange("p (jj q) -> p jj q", q=128), AF.Exp,
                              [f"ps{2 * sbk + half}"], [ek], scale=0.125, nosame=(half == 1))
                    if msk is not None:
                        C.tt("pool", E_[:, :], E_[:, :], mk[:, msk, :], ALU.mult, [ek, "w_mask"], [ek])
                    for j in range(4):
                        C.amm(4 + kh, pO[:, j, :], E_[:, j * 128:(j + 1) * 128], Vt[:, kt, kh, :], [ek, "w_V"], [pok])
                    ecnt += 1
                C.adone(4 + kh)
                C.tt("dve", d_[:, kh * 4:(kh + 1) * 4], pO[:, :, 64], sk[:, kh * 4:(kh + 1) * 4], ALU.add, [pok, "w_sink"], [dk])
                C.recip(d_[:, kh * 4:(kh + 1) * 4], d_[:, kh * 4:(kh + 1) * 4], [dk], [dk])
                C.tt("dve", y_[:, kh * 256:(kh + 1) * 256].rearrange("p (j d) -> p j d", d=64), pO[:, :, 0:64],
                     d_[:, kh * 4:(kh + 1) * 4].unsqueeze(2).to_broadcast([128, 4, 64]), ALU.mult, [pok, dk], [yk])
            emit_y(y_[:, :], 1, tq, yk)
        sb.release(m0)

    def na_keys(tq):
        n = tq - NCT
        r0 = 2 * n
        s0 = min(max(r0 - 4, 0), 24); s1 = min(max(r0 + 1 - 4, 0), 24)
        first = s0 // 2; last = (s1 + 7) // 2
        typ = 0 if n == 0 else 1 if n == 1 else 3 if n == 14 else 4 if n == 15 else 2
        return [(kt + NCT, kt - first) for kt in range(first, last + 1)], typ

    def phase_na(l):
        m0 = sb.mark()
        qT = sb.alloc("n_qT", [128, 4, T], BF16)
        kT = sb.alloc("n_kT", [128, 4, T], BF16)
        Vt = sb.alloc("n_V", [128, NT, 8, 65], BF16)
        load_v(3840, 8, 64, Vt, "n_V")
        prep_qk(l, 2816, 8, 4, False, qT, qscale=0.125, tag="n_q")
        prep_qk(l, 3328, 8, 5, False, kT, tag="n_k")
        alloc_y()
        nb = [sb.alloc(f"n_b{i}", [128, 8, 5, 128], BF16) for i in range(2)]
        E = [sb.alloc(f"n_E{i}", [128, 7, 128], BF16) for i in range(2)]
        ysb = [sb.alloc(f"n_y{i}", [128, 512], F32) for i in range(2)]
        den = [sb.alloc(f"n_den{i}", [128, 8], F32) for i in range(2)]
        allq = [("n_q", t) for t in range(NT)]; allk = [("n_k", t) for t in range(NT)]
        ecnt = 0
        for tq in range(NT):
            y_ = ysb[tq % 2]; yk = f"n_y{tq % 2}"
            d_ = den[tq % 2]; dk = f"n_den{tq % 2}"
            if tq < NCT:
                keys = [(0, None), (1, None)]
            else:
                loc, typ = na_keys(tq)
                keys = [(kt, sl) for kt, sl in loc] + [(0, None), (1, None)]
                b_ = nb[tq % 2]; bk = f"n_b{tq % 2}"
                p.dma("pool", [lambda e, b_=b_, typ=typ: e.dma_start(
                    out=b_[:, :, :, :], in_=I["nab"][l, typ, :, :].rearrange("p (h s k) -> p h s k", h=8, s=5))], bk, [], [bk])
            nk = len(keys)
            for h in range(8):
                half = h % 2; hp = h // 2
                set_ = ecnt % 2
                bA = PS[2 * set_]; bB = PS[2 * set_ + 1]
                for i_, (kt, sl) in enumerate(keys):
                    bank = bA if i_ < 4 else bB
                    bkey = f"ps{2 * set_ + (0 if i_ < 4 else 1)}"
                    reg = bank[:, (i_ % 4) * 128:(i_ % 4 + 1) * 128]
                    C.mm(reg, kT[half * 64:(half + 1) * 64, hp, kt * 128:(kt + 1) * 128],
                         qT[half * 64:(half + 1) * 64, hp, tq * 128:(tq + 1) * 128], True, sl is None, allq + allk, [bkey])
                    if sl is not None:
                        C.mm(reg, b_[:, h, sl, :], identb[:, :], False, True, [bk, "identb"], [bkey])
                E_ = E[set_]; ek = f"n_E{set_}"
                nA = min(4, nk)
                C.act(E_[:, 0:nA, :], bA[:, 0:nA * 128].rearrange("p (a b) -> p a b", b=128), AF.Exp, [f"ps{2 * set_}"], [ek])
                if nk > 4:
                    C.act(E_[:, 4:nk, :], bB[:, 0:(nk - 4) * 128].rearrange("p (a b) -> p a b", b=128), AF.Exp,
                          [f"ps{2 * set_ + 1}"], [ek], nosame=True)
                pob = 4 + (h // 4)
                pO = PS[pob][:, 0:260].rearrange("p (j c) -> p j c", c=65)
                for i_, (kt, sl) in enumerate(keys):
                    C.amm(pob, pO[:, h % 4, :], E_[:, i_, :], Vt[:, kt, h, :], [ek, "n_V"], [f"ps{pob}"])
                ecnt += 1
                if h % 4 == 3:
                    C.adone(pob)
                    g = h // 4
                    C.recip(d_[:, g * 4:(g + 1) * 4], pO[:, :, 64], [f"ps{pob}"], [dk])
                    C.tt("dve", y_[:, g * 256:(g + 1) * 256].rearrange("p (j d) -> p j d", d=64), pO[:, :, 0:64],
                         d_[:, g * 4:(g + 1) * 4].unsqueeze(2).to_broadcast([128, 4, 64]), ALU.mult, [f"ps{pob}", dk], [yk])
            emit_y(y_[:, :], 3, tq, yk)
        sb.release(m0)

    def phase_diff(l, lam_init):
        m0 = sb.mark()
        qT = sb.alloc("d_qT", [128, 4, T], BF16)
        kT = sb.alloc("d_kT", [128, 4, T], BF16)
        Vt = sb.alloc("d_V", [128, NT, 4, 129], BF16)
        load_v(2304, 4, 128, Vt, "d_V")
        lp = sb.alloc("d_lp", [128, 4, 64], F32)
        lw = sb.alloc("d_lw", [128, 8], F32)
        sg = sb.alloc("d_sg", [128, 128], F32)
        bcast_load(lp[:, :, :].rearrange("p a d -> p (a d)"), I["dlam"][l, :], 256, "d_lp", "d_lp")
        bcast_load(sg[:, :], I["subg"][l, :], 128, "d_sg", "d_sg")
        C.ts("dve", sg[:, :], sg[:, :], 1.0 - lam_init, None, ALU.mult, None, ["d_sg"], ["d_sg"])
        C.tt("dve", lp[:, 0, :], lp[:, 0, :], lp[:, 1, :], ALU.mult, ["d_lp"], ["d_lp"])
        C.tt("dve", lp[:, 2, :], lp[:, 2, :], lp[:, 3, :], ALU.mult, ["d_lp"], ["d_lp"])
        C.red(lw[:, 0:1], lp[:, 0, :], ALU.add, ["d_lp"], ["d_lw"])
        C.red(lw[:, 1:2], lp[:, 2, :], ALU.add, ["d_lp"], ["d_lw"])
        C.act(lw[:, 2:4], lw[:, 0:2], AF.Exp, ["d_lw"], ["d_lw"])
        C.tt("dve", lw[:, 4:5], lw[:, 3:4], lw[:, 2:3], ALU.subtract, ["d_lw"], ["d_lw"])
        C.ts("dve", lw[:, 5:6], lw[:, 4:5], -lam_init, None, ALU.add, None, ["d_lw"], ["d_lw"])
        prep_qk(l, 1280, 8, 2, True, qT, tag="d_q")
        prep_qk(l, 1792, 8, 3, True, kT, tag="d_k")
        alloc_y()
        E = [sb.alloc(f"d_E{i}", [128, 512], BF16) for i in range(2)]
        ybuf = sb.alloc("d_ybuf", [128, 4, 512], F32)
        o1 = [sb.alloc(f"d_o1{i}", [128, 128], F32) for i in range(2)]
        junk = sb.alloc("d_junk", [128, 128], F32)
        dst_ = [sb.alloc(f"d_st{i}", [128, 8], F32) for i in range(2)]
        allq = [("d_q", t) for t in range(NT)]; allk = [("d_k", t) for t in range(NT)]
        chunks = [[0, 1]] + [[2 + 4 * c + j for j in range(4)] for c in range(4)]
        ecnt = 0; ocnt = 0
        def preg(m, j):
            r = m * 4 + j
            return PS[4 + r // 3][:, (r % 3) * 129:(r % 3 + 1) * 129], f"ps{4 + r // 3}"
        for ch in chunks:
            keys = [0, 1] if ch[0] < NCT else list(range(NT))
            nq = len(ch) * 128
            c0 = ch[0] * 128
            for h in range(4):
                for m in range(2):
                    for ki, kt in enumerate(keys):
                        sbk = ecnt % 2
                        bank = PS[sbk]
                        C.mm(bank[:, 0:nq], kT[m * 64:(m + 1) * 64, h, kt * 128:(kt + 1) * 128],
                             qT[m * 64:(m + 1) * 64, h, c0:c0 + nq], True, True, allq + allk, [f"ps{sbk}"])
                        E_ = E[sbk]; ek = f"d_E{sbk}"
                        C.act(E_[:, 0:nq], bank[:, 0:nq], AF.Exp, [f"ps{sbk}"], [ek], scale=0.125)
                        for j in range(len(ch)):
                            reg, rk = preg(m, j)
                            C.amm(int(rk[2:]), reg, E_[:, j * 128:(j + 1) * 128], Vt[:, kt, h, :], [ek, "d_V"], [rk])
                        ecnt += 1
                for bnk in (4, 5, 6):
                    C.adone(bnk)
                for j in range(len(ch)):
                    r0_, k0 = preg(0, j); r1_, k1 = preg(1, j)
                    st_ = dst_[ocnt % 2]; sk_ = f"d_st{ocnt % 2}"
                    o_ = o1[ocnt % 2]; ok_ = f"d_o1{ocnt % 2}"
                    C.recip(st_[:, 0:1], r0_[:, 128:129], [k0], [sk_])
                    C.recip(st_[:, 1:2], r1_[:, 128:129], [k1], [sk_])
                    C.tt("dve", st_[:, 2:3], st_[:, 1:2], lw[:, 5:6], ALU.mult, [sk_, "d_lw"], [sk_])
                    C.ts("dve", o_[:, :], r0_[:, 0:128], st_[:, 0:1], None, ALU.mult, None, [k0, sk_], [ok_])
                    C.stt(o_[:, :], r1_[:, 0:128], st_[:, 2:3], o_[:, :], ALU.mult, ALU.add, [k1, sk_, ok_], [ok_])
                    C.act(junk[:, :], o_[:, :], AF.Square, [ok_], ["d_junk", sk_], accum=st_[:, 3:4])
                    C.ts("dve", st_[:, 4:5], st_[:, 3:4], 1.0 / 128, EPS, ALU.mult, ALU.add, [sk_], [sk_])
                    C.act(st_[:, 5:6], st_[:, 4:5], AF.Sqrt, [sk_], [sk_])
                    C.recip(st_[:, 6:7], st_[:, 5:6], [sk_], [sk_])
                    C.stt(ybuf[:, j, h * 128:(h + 1) * 128], o_[:, :], st_[:, 6:7], sg[:, :], ALU.mult, ALU.mult,
                          [ok_, sk_, "d_sg"], [("d_ybuf", j)])
                    ocnt += 1
            for j, tq in enumerate(ch):
                emit_y(ybuf[:, j, :], 2, tq, ("d_ybuf", j))
        sb.release(m0)

    def phase_s5(l):
        NK = 288
        KT = [(0, 128), (128, 128), (256, 32)]
        mP = sb.mark()
        GL = sb.alloc("s5_GL", [128, 32, 2, 128], BF16)
        KS = sb.alloc("s5_KS", [128, 32, 128], BF16)
        HH = sb.alloc("s5_HH", [128, 32, 2, 128], BF16)
        A8 = sb.alloc("s5_A8", [128, 2, 2, 32], F32)
        mS = sb.mark()
        a = sb.alloc("s5_a", [128, 32, 2], F32)
        w = sb.alloc("s5_w", [128, 24, 32], F32)
        Bm = sb.alloc("s5_B", [128, 32, 2, 16], F32)
        Cm = sb.alloc("s5_C", [128, 32, 2, 16], F32)
        Bb = sb.alloc("s5_Bb", [128, 2, 32, 16], F32)
        Pp = sb.alloc("s5_Pp", [128, 2, 32, 9], F32)
        Pn = sb.alloc("s5_Pn", [128, 2, 32, 8], F32)
        PG = sb.alloc("s5_PG", [128, 3, 2, 32, 8], F32)
        Gf = sb.alloc("s5_Gf", [128, 2, 32, 128], F32)
        Rf = sb.alloc("s5_Rf", [128, 2, 32, 128], F32)
        t1 = sb.alloc("s5_t1", [128, 32, 128], F32)
        t2 = sb.alloc("s5_t2", [128, 32, 128], F32)
        msk = sb.alloc("s5_msk", [128, 2, 128], F32)
        dd = sb.alloc("s5_dd", [128, 32], F32)
        K = "s5set"
        C.dma("sp", a[:, :, :], I["s5a"][l], "s5_a", [], [K])
        C.dma("sp", w[:, 0, :], I["s5ls"][l], "s5_ls", [], [K])
        C.dma("sp", Bm[:, :, :, :], I["s5b"][l], "s5_b", [], [K])
        C.dma("sp", Cm[:, :, :, :], I["s5c"][l], "s5_c", [], [K])
        C.dma("sp", dd[:, :], I["s5d"][l], "s5_d", [], [K])
        C.dma("sp", msk[:, :, :], I["s5mask"].rearrange("m p c -> p m c"), "s5_m", [], [K])
        W = lambda i: w[:, i, :]
        are = a[:, :, 0]; aim = a[:, :, 1]
        def TT(o, x, y, op): C.tt("dve", o, x, y, op, [K], [K])
        def TS(o, x, s1, s2, op0, op1): C.ts("dve", o, x, s1, s2, op0, op1, [K], [K])
        def ACT(o, x, f, scale=1.0): C.act(o, x, f, [K], [K], scale=scale)
        ACT(W(0), W(0), AF.Exp)
        TT(W(1), are, W(0), ALU.mult)
        TT(W(2), aim, W(0), ALU.mult)
        ACT(W(3), W(1), AF.Exp)
        ACT(W(4), W(1), AF.Exp, scale=-1.0)
        ACT(W(5), W(2), AF.Sin, scale=1.0 / 32)
        ACT(W(6), W(2), AF.Sin, scale=1.0 / 16)
        TT(W(7), W(5), W(5), ALU.mult)
        TS(W(7), W(7), -2.0, 1.0, ALU.mult, ALU.add)
        for _ in range(4):
            TT(W(8), W(7), W(7), ALU.mult)
            TT(W(9), W(6), W(6), ALU.mult)
            TT(W(10), W(7), W(6), ALU.mult)
            TT(W(7), W(8), W(9), ALU.subtract)
            TS(W(6), W(10), 2.0, None, ALU.mult, None)
        Lr, Li, Ir, Ii = W(11), W(12), W(13), W(14)
        TT(Lr, W(3), W(7), ALU.mult); TT(Li, W(3), W(6), ALU.mult)
        TT(Ir, W(4), W(7), ALU.mult); TT(Ii, W(4), W(6), ALU.mult)
        TS(Ii, Ii, -1.0, None, ALU.mult, None)
        TS(W(15), Lr, -1.0, None, ALU.add, None)
        TT(W(16), W(15), are, ALU.mult); TT(W(17), Li, aim, ALU.mult); TT(W(16), W(16), W(17), ALU.add)
        TT(W(17), Li, are, ALU.mult); TT(W(18), W(15), aim, ALU.mult); TT(W(17), W(17), W(18), ALU.subtract)
        TT(W(18), are, are, ALU.mult); TT(W(19), aim, aim, ALU.mult); TT(W(18), W(18), W(19), ALU.add)
        p.op("dve", lambda e: e.reciprocal(out=W(18), in_=W(18)), [K], [K])
        TT(W(16), W(16), W(18), ALU.mult); TT(W(17), W(17), W(18), ALU.mult)
        gr = W(16).unsqueeze(2).to_broadcast([128, 32, 16]); gi = W(17).unsqueeze(2).to_broadcast([128, 32, 16])
        Br = Bm[:, :, 0, :]; Bi = Bm[:, :, 1, :]
        T1 = t1[:, :, 0:16]; T2 = t2[:, :, 0:16]
        TT(T1, Br, gr, ALU.mult); TT(T2, Bi, gi, ALU.mult); TT(Bb[:, 0, :, :], T1, T2, ALU.subtract)
        TT(T1, Bi, gr, ALU.mult); TT(T2, Br, gi, ALU.mult); TT(Bb[:, 1, :, :], T1, T2, ALU.add)
        C.memset("dve", Pp[:, 0, :, 0:1], 1.0, [K]); C.memset("dve", Pp[:, 1, :, 0:1], 0.0, [K])
        C.memset("dve", Pn[:, 0, :, 0:1], 1.0, [K]); C.memset("dve", Pn[:, 1, :, 0:1], 0.0, [K])
        def cpow(P, n, Xr, Xi):
            for s_ in range(1, n):
                pr, pi = P[:, 0, :, s_ - 1], P[:, 1, :, s_ - 1]
                TT(W(20), pr, Xr, ALU.mult); TT(W(21), pi, Xi, ALU.mult); TT(P[:, 0, :, s_], W(20), W(21), ALU.subtract)
                TT(W(20), pr, Xi, ALU.mult); TT(W(21), pi, Xr, ALU.mult); TT(P[:, 1, :, s_], W(20), W(21), ALU.add)
        cpow(Pp, 9, Lr, Li)
        cpow(Pn, 8, Ir, Ii)
        for ri_ in range(2):
            C.cp("dve", A8[:, 0, ri_, :], Pp[:, 0, :, 8], [K], [K])
        TS(A8[:, 1, 0, :], Pp[:, 1, :, 8], -1.0, None, ALU.mult, None)
        C.cp("dve", A8[:, 1, 1, :], Pp[:, 1, :, 8], [K], [K])
        for ri_ in range(2):
            C.cp("dve", PG[0:64, 0, ri_, :, :], Pp[0:64, ri_, :, 7::-1], [K], [K])
            C.cp("dve", PG[64:128, 0, ri_, :, :], Pp[64:128, ri_, :, 0:8], [K], [K])
            C.cp("dve", PG[0:64, 1, ri_, :, :], Pp[0:64, ri_, :, 1:9], [K], [K])
            C.cp("dve", PG[64:128, 1, ri_, :, :], Pp[64:128, ri_, :, 8:0:-1], [K], [K])
            C.cp("dve", PG[0:64, 2, ri_, :, :], Pn[0:64, ri_, :, 7::-1], [K], [K])
            C.cp("dve", PG[64:128, 2, ri_, :, :], Pn[64:128, ri_, :, 0:8], [K], [K])
        def outer(dst_r, dst_i, tb, Mr, Mi, neg_i):
            pr = PG[:, tb, 0, :, :].unsqueeze(3).to_broadcast([128, 32, 8, 16])
            pi = PG[:, tb, 1, :, :].unsqueeze(3).to_broadcast([128, 32, 8, 16])
            mr = Mr.unsqueeze(2).to_broadcast([128, 32, 8, 16]); mi = Mi.unsqueeze(2).to_broadcast([128, 32, 8, 16])
            A_ = t1[:, :, :].rearrange("p g (s c) -> p g s c", c=16); B_ = t2[:, :, :].rearrange("p g (s c) -> p g s c", c=16)
            TT(A_, pr, mr, ALU.mult); TT(B_, pi, mi, ALU.mult); TT(dst_r, A_, B_, ALU.subtract)
            TT(A_, pr, mi, ALU.mult); TT(B_, pi, mr, ALU.mult)
            TT(dst_i, A_, B_, ALU.add)
            if neg_i:
                TS(dst_i, dst_i, -1.0, None, ALU.mult, None)
        v4 = lambda x: x.rearrange("p g (s c) -> p g s c", c=16)
        outer(v4(Gf[:, 0, :, :]), v4(Gf[:, 1, :, :]), 0, Bb[:, 0, :, :], Bb[:, 1, :, :], False)
        outer(v4(Rf[:, 0, :, :]), v4(Rf[:, 1, :, :]), 2, Cm[:, :, 0, :], Cm[:, :, 1, :], True)
        for g in range(32):
            pb = g % 2
            bank = PS[pb]; bk = f"ps{pb}"
            bankb = PS[4 + pb]; bkb = f"ps{4 + pb}"
            for d_ in range(2):
                sl = slice(64 * d_, 64 * (d_ + 1))
                reg = (bank if d_ == 0 else bankb)[:, 0:128]
                C.mm(reg, Gf[sl, 0, g, :], Rf[sl, 0, g, :], True, False, [K], [bk if d_ == 0 else bkb])
                C.mm(reg, Gf[sl, 1, g, :], Rf[sl, 1, g, :], False, True, [K], [bk if d_ == 0 else bkb])
            ks = t1[:, g, :]
            C.tt("dve", ks, bank[:, 0:128], msk[:, 0, :], ALU.mult, [bk, K], [K])
            C.tt("dve", t2[:, g, :], bankb[:, 0:128], msk[:, 1, :], ALU.mult, [bkb, K], [K])
            C.tt("dve", ks, ks, t2[:, g, :], ALU.add, [K], [K])
            C.stt(KS[:, g, :], ident[:, :], dd[:, g:g + 1], ks, ALU.mult, ALU.add, ["ident", K], ["s5_KS", K])
            pb2 = 2 + g % 2
            pT = PS[pb2][:, 0:256].rearrange("p (a b) -> p a b", b=128)
            for ri_ in range(2):
                C.tr(pT[:, ri_, :], Gf[:, ri_, g, :], ident[:, :], [K, "ident"], [f"ps{pb2}"])
            C.act(GL[:, g, :, :], pT[:, :, :], AF.Copy, [f"ps{pb2}"], ["s5_GL"])
        Hs = Rf
        outer(v4(Hs[:, 0, :, :]), v4(Hs[:, 1, :, :]), 1, Cm[:, :, 0, :], Cm[:, :, 1, :], True)
        for ri_ in range(2):
            C.cp("dve", HH[:, :, ri_, :], Hs[:, ri_, :, :], [K], ["s5_HH"])
        sb.release(mS)
        uB = sb.alloc("s5_uB", [128, 32, NK], BF16)
        mS = sb.mark()
        ubs = sb.alloc("s5_ubs", [128, 8, 512], F32)
        ubp = sb.alloc("s5_ubp", [128, 32, 128], F32)
        for (k0, kn) in KT:
            C.dma("sp", ubs[0:kn, :, :], PROJ[8 * k0:8 * (k0 + kn), 0:512].rearrange("(k t) c -> k t c", t=8), "s5_ubs",
                  [("PROJ", t_, c_) for t_ in range(NT) for c_ in range(9)], ["s5_ubs"])
            C.cp("dve", ubp[0:kn, :, :].rearrange("k g (t c) -> k g t c", c=16),
                 ubs[0:kn, :, :].rearrange("k t (g c) -> k g t c", c=16), ["s5_ubs"], ["s5_ubp"])
            for g4 in range(8):
                pb = 2 + g4 % 2
                pT = PS[pb][:, :].rearrange("p (a b) -> p a b", b=128)
                for a_ in range(4):
                    g = g4 * 4 + a_
                    C.tr(pT[:, a_, 0:kn], ubp[0:kn, g, :], ident[0:kn, 0:kn], ["s5_ubp", "ident"], [f"ps{pb}"])
                C.act(uB[:, g4 * 4:(g4 + 1) * 4, k0:k0 + kn], pT[:, :, 0:kn], AF.Copy, [f"ps{pb}"], ["s5_uB"], nosame=True)
        sb.release(mS)
        X = sb.alloc("s5_X", [128, 2, 32, NK], F32)
        for g in range(32):
            pb = g % 2
            for ri_ in range(2):
                C.mm(PS[2 * pb + ri_][:, 0:NK], GL[:, g, ri_, :], uB[:, g, :], True, True, ["s5_GL", "s5_uB"], [f"ps{2 * pb + ri_}"])
                C.cp("act" if ri_ == 0 else "dve", X[:, ri_, g, :], PS[2 * pb + ri_][:, 0:NK], [f"ps{2 * pb + ri_}"], ["s5_X0"])
        sT = [sb.alloc(f"s5_sT{i}", [128, 2, 2, 32], F32) for i in range(1)][0]
        def step(eng, sl, k, kp, key):
            Z = X[sl, :, :, kp]; Zs = X[sl, ::-1, :, kp]
            C.tt(eng, sT[sl, 0, :, :], A8[sl, 0, :, :], Z, ALU.mult, [key, "s5_X0", K], [key + "a"])
            C.tt(eng, sT[sl, 1, :, :], A8[sl, 1, :, :], Zs, ALU.mult, [key, "s5_X0", K], [key + "b"])
            C.tt(eng, sT[sl, 0, :, :], sT[sl, 0, :, :], sT[sl, 1, :, :], ALU.add, [key + "a", key + "b"], [key + "a"])
            C.tt(eng, X[sl, :, :, k], X[sl, :, :, k], sT[sl, 0, :, :], ALU.add, [key + "a", key, "s5_X0"], [key])
        for k in range(1, NK):
            step("dve", slice(0, 64), k, k - 1, "s5_Xf")
        border = [(k, k + 1) for k in range(30, -1, -1)] + [(287, 0)] + [(k, k + 1) for k in range(286, 31, -1)]
        for (k, kp) in border:
            step("pool", slice(64, 128), k, kp, "s5_Xb")
        XpB = sb.alloc("s5_XpB", [128, 2, 32, NK], BF16)
        xr = ["s5_Xf", "s5_Xb", "s5_X0"]
        C.memset("dve", XpB[0:64, :, :, 0:1], 0.0, ["s5_XpBf"])
        C.cp("dve", XpB[0:64, :, :, 1:NK], X[0:64, :, :, 0:NK - 1], xr, ["s5_XpBf"])
        C.cp("pool", XpB[64:128, :, :, 0:NK - 1], X[64:128, :, :, 1:NK], xr, ["s5_XpBb"])
        C.memset("pool", XpB[64:128, :, :, 31:32], 0.0, ["s5_XpBb"])
        C.cp("pool", XpB[64:128, :, :, NK - 1:NK], X[64:128, :, :, 0:1], xr + ["s5_XpBb"], ["s5_XpBb"])
        yt_ = sb.alloc("s5_yt", [128, 8, 32, 16], F32)
        cnt = 0
        for (k0, kn) in KT:
            for g4 in range(8):
                pb = cnt % 2; cnt += 1
                for a_ in range(4):
                    g = g4 * 4 + a_
                    reg = PS[pb][0:kn, a_ * 128:(a_ + 1) * 128]
                    C.mm(reg, uB[:, g, k0:k0 + kn], KS[:, g, :], True, False, ["s5_uB", "s5_KS"], [f"ps{pb}"])
                    C.mm(reg, XpB[:, 0, g, k0:k0 + kn], HH[:, g, 0, :], False, False, ["s5_XpBf", "s5_XpBb", "s5_HH"], [f"ps{pb}"])
                    C.mm(reg, XpB[:, 1, g, k0:k0 + kn], HH[:, g, 1, :], False, True, ["s5_XpBf", "s5_XpBb", "s5_HH"], [f"ps{pb}"])
                C.cp("act" if g4 % 2 else "dve", yt_[0:kn, :, g4 * 4:(g4 + 1) * 4, :].rearrange("k i g c -> k g i c"),
                     PS[pb][0:kn, :].rearrange("k (g i c) -> k g i c", i=8, c=16), [f"ps{pb}"], ["s5_yt"], )
            C.dma("sp", YS[8 * k0:8 * (k0 + kn), :].rearrange("(k i) c -> k i c", i=8), yt_[0:kn, :, :, :].rearrange("k i g c -> k i (g c)"),
                  "s5_yt", ["s5_yt"], [("YS", k0)])
        sb.release(mP)
        wg = sb.alloc("s5_wg", [128, 4, 512], BF16)
        bg = sb.alloc("s5_bg", [128, 512], F32)
        p.dma("pool", [lambda e: e.dma_start(out=wg[:, :, :], in_=I["s5_w_glu"][l].rearrange("(kc p) n -> p kc n", p=128))],
              "s5_wg", [], ["s5_wg"])
        bcast_load(bg[:, :], I["s5_b_glu"][l, :], 512, "s5_bg", "s5_bg")
        alloc_y()
        ysb = [sb.alloc(f"s5_y{i}", [128, 512], F32) for i in range(2)]
        ug = [sb.alloc(f"s5_u{i}", [128, 512], F32) for i in range(2)]
        ygb = [sb.alloc(f"s5_ygb{i}", [128, 512], BF16) for i in range(2)]
        ygT = [sb.alloc(f"s5_ygT{i}", [128, 4, 128], BF16) for i in range(2)]
        ysall = [("YS", k0) for (k0, kn) in KT]
        for tt in range(NT):
            i = tt % 2
            y_, u_, gb_, gT_ = ysb[i], ug[i], ygb[i], ygT[i]
            ky, ku, kg, kt_ = f"s5_y{i}", f"s5_u{i}", f"s5_ygb{i}", f"s5_ygT{i}"
            C.dma("sp", y_[:, :], YS[tt * 128:(tt + 1) * 128, :], ky, ysall, [ky])
            C.tt("dve", u_[:, :], y_[:, :], y_[:, :], ALU.mult, [ky], [ku])
            C.ts("dve", u_[:, :], u_[:, :], 0.044715, 1.0, ALU.mult, ALU.add, [ku], [ku])
            C.tt("dve", u_[:, :], u_[:, :], y_[:, :], ALU.mult, [ku, ky], [ku])
            C.act(u_[:, :], u_[:, :], AF.Sigmoid, [ku], [ku], scale=2.0 * math.sqrt(2.0 / math.pi))
            C.tt("dve", y_[:, :], y_[:, :], u_[:, :], ALU.mult, [ku, ky], [ky])
            C.cp("dve", gb_[:, :], y_[:, :], [ky], [kg])
            pTb = PS[7][:, :].bitcast(BF16).rearrange("p (a b) -> p a b", b=128)
            for a_ in range(4):
                C.tr(pTb[:, a_, :], gb_[:, a_ * 128:(a_ + 1) * 128], identb[:, :], [kg, "identb"], ["ps7"])
            C.cp("act", gT_[:, :, :], pTb[:, 0:4, :], ["ps7"], [kt_])
            pb = i
            for kc in range(4):
                C.mm(PS[pb][:, :], gT_[:, kc, :], wg[:, kc, :], kc == 0, kc == 3, [kt_, "s5_wg"], [f"ps{pb}"])
            C.tt("dve", u_[:, :], PS[pb][:, :], bg[:, :], ALU.add, [f"ps{pb}", "s5_bg", ku], [ku])
            C.act(u_[:, :], u_[:, :], AF.Sigmoid, [ku], [ku])
            C.tt("dve", y_[:, :], y_[:, :], u_[:, :], ALU.mult, [ku, ky], [ky])
            emit_y(y_[:, :], 0, tt, ky)
        sb.release(mP)

    TCH = [(0, 512), (512, 512), (1024, 512), (1536, 512), (2048, 256)]

    def build_gateB(gB, j):
        dg = [sb.alloc(f"gdiag{i}", [128, 128], F32) for i in range(2)]
        cnt = 0
        for lc in range(2):
            for k4 in range(4):
                pb = 2 + k4 % 2
                for a_ in range(4):
                    kc = k4 * 4 + a_
                    d_ = dg[cnt % 2]; dk = f"gdiag{cnt % 2}"; cnt += 1
                    C.ts("dve", d_[:, :], ident[:, :], modT[:, j * 16 + kc, lc:lc + 1], None, ALU.mult, None, ["ident", "modT"], [dk])
                    C.mm(PS[pb][:, a_ * 128:(a_ + 1) * 128], ones[:, :], d_[:, :], True, True, [dk, "ones"], [f"ps{pb}"])
                C.cp("act", gB[:, lc, k4 * 512:(k4 + 1) * 512], PS[pb][:, :], [f"ps{pb}"], ["gateB"], )

    def phase_merge(l):
        m0 = sb.mark()
        wbr = sb.alloc("wbr", [128, 16, 2048], BF16)
        wsrc = I["w_branch"][l].rearrange("(k p) n -> p k n", p=128)
        for k4 in range(4):
            p.dma("pool", [lambda e, k4=k4: e.dma_start(out=wbr[:, k4 * 4:(k4 + 1) * 4, :], in_=wsrc[:, k4 * 4:(k4 + 1) * 4, :])],
                  "wbr", [], ["wbr"])
        ytc = [sb.alloc(f"ytc{i}", [128, 16, 512], BF16) for i in range(2)]
        sgb = [sb.alloc(f"sgb{i}", [128, 4, 512], BF16) for i in range(2)]
        acc = [sb.alloc(f"macc{i}", [128, 512], F32) for i in range(2)]
        tmp = [sb.alloc(f"mtmp{i}", [128, 512], F32) for i in range(2)]
        mgo = [sb.alloc(f"mgo{i}", [128, 512], BF16) for i in range(2)]
        SGv = SG.rearrange("(i f) p t -> f p i t", i=4)
        yall = [("YT", b, t) for b in range(4) for t in range(NT)]
        sgall = [("SG", gi) for gi in range(64)]
        cnt = 0; fcnt = 0
        for ci, (t0, tw) in enumerate(TCH):
            y_ = ytc[ci % 2]; yk = f"ytc{ci % 2}"
            C.dma("sp", y_[:, :, 0:tw], YT[:, :, t0:t0 + tw].rearrange("k p t -> p k t"), yk, yall, [yk])
            for fc in range(16):
                s_ = sgb[fcnt % 2]; sk = f"sgb{fcnt % 2}"
                a_ = acc[fcnt % 2]; ak = f"macc{fcnt % 2}"
                o_ = mgo[fcnt % 2]; ok_ = f"mgo{fcnt % 2}"
                fcnt += 1
                C.dma("sp", s_[:, :, 0:tw], SGv[fc][:, :, t0:t0 + tw], sk, sgall, [sk])
                for i in range(4):
                    pb = cnt % 2
                    t_ = tmp[cnt % 2]; tk = f"mtmp{cnt % 2}"
                    cnt += 1
                    for kc in range(4):
                        C.mm(PS[pb][:, 0:tw], wbr[:, i * 4 + kc, fc * 128:(fc + 1) * 128], y_[:, i * 4 + kc, 0:tw], kc == 0, kc == 3,
                             ["wbr", yk], [f"ps{pb}"])
                    if i == 0:
                        C.tt("dve", a_[:, 0:tw], PS[pb][:, 0:tw], s_[:, i, 0:tw], ALU.mult, [f"ps{pb}", sk], [ak])
                    else:
                        C.tt("dve", t_[:, 0:tw], PS[pb][:, 0:tw], s_[:, i, 0:tw], ALU.mult, [f"ps{pb}", sk], [tk])
                        if i < 3:
                            C.tt("pool", a_[:, 0:tw], a_[:, 0:tw], t_[:, 0:tw], ALU.add, [ak, tk], [ak])
                        else:
                            C.tt("pool", o_[:, 0:tw], a_[:, 0:tw], t_[:, 0:tw], ALU.add, [ak, tk], [ok_])
                C.dma("sp", MG[fc, :, t0:t0 + tw], o_[:, 0:tw], ok_, [ok_], [("MG", fc, ci)])
        sb.release(m0)

    def resid_update(src_ap, gB, tt, nb, srckeys, evs, cnt):
        lc = tok_lc(tt)
        e_ = evs[cnt % 2]; ek = f"rev{cnt % 2}"
        C.tt("dve", e_[:, :], src_ap, gB[:, lc, nb * 512:(nb + 1) * 512], ALU.mult, srckeys + ["gateB"], [ek])
        p.dma("pool", [lambda e: e.dma_start(out=XR[tt * 128:(tt + 1) * 128, nb * 512:(nb + 1) * 512], in_=e_[:, :], accum_op=ALU.add)],
              ek, [ek, ("XR", tt)], [("XR", tt)])

    def phase_wout(l):
        m0 = sb.mark()
        wo = sb.alloc("wo", [128, 16, 2048], BF16)
        wsrc = I["w_out"][l].rearrange("(k p) n -> p k n", p=128)
        for k4 in range(4):
            p.dma("pool", [lambda e, k4=k4: e.dma_start(out=wo[:, k4 * 4:(k4 + 1) * 4, :], in_=wsrc[:, k4 * 4:(k4 + 1) * 4, :])],
                  "wo", [], ["wo"])
        gB = sb.alloc("gateB", [128, 2, 2048], F32)
        build_gateB(gB, 2)
        mgt = [sb.alloc(f"mgt{i}", [128, 16, 128], BF16) for i in range(2)]
        evs = [sb.alloc(f"rev{i}", [128, 512], F32) for i in range(2)]
        mgall = [("MG", fc, ci) for fc in range(16) for ci in range(5)]
        cnt = 0
        for tt in range(NT):
            m_ = mgt[tt % 2]; mk_ = f"mgt{tt % 2}"
            C.dma("sp", m_[:, :, :], MG[:, :, tt * 128:(tt + 1) * 128].rearrange("k p t -> p k t"), mk_, mgall, [mk_])
            for nb in range(4):
                pb = cnt % 2
                for fc in range(16):
                    C.mm(PS[pb][:, :], m_[:, fc, :], wo[:, fc, nb * 512:(nb + 1) * 512], fc == 0, fc == 15, [mk_, "wo"], [f"ps{pb}"])
                resid_update(PS[pb][:, :], gB, tt, nb, [f"ps{pb}"], evs, cnt)
                cnt += 1
        sb.release(m0)

    def phase_router_norm(l, hT):
        m0 = sb.mark()
        xt = [sb.alloc(f"xt{i}", [128, D], F32) for i in range(2)]
        junk = sb.alloc("junk", [128, D], BF16)
        st = [sb.alloc(f"nst{i}", [128, 4], F32) for i in range(2)]
        hf = [sb.alloc(f"hf{i}", [128, 16, 128], F32) for i in range(2)]
        wr = sb.alloc("wr", [128, 16, 20], F32)
        br = sb.alloc("br", [128, 20], F32)
        cw = sb.alloc("cw", [128, NT, 16], F32)
        rt = [sb.alloc(f"rt{i}", [128, 96], F32) for i in range(2)]
        C.dma("sp", wr[:, :, :], I["wrT"][l].rearrange("p (k n) -> p k n", n=20), "wr", [], ["wr"])
        bcast_load(br[:, :], I["brt"][l, :], 20, "br", "br")
        s = 1
        for tt in range(NT):
            i = tt % 2
            lc = tok_lc(tt)
            x_, s_, h_ = xt[i], st[i], hf[i]
            hk = f"hf{i}"
            C.dma("sp", x_[:, :], XR[tt * 128:(tt + 1) * 128, :], f"xt{i}", [("XR", tt)], [f"xt{i}"])
            C.act(junk[:, :], x_[:, :], AF.Square, [f"xt{i}"], ["junk", f"nst{i}a"], accum=s_[:, 0:1])
            C.ts("dve", s_[:, 1:2], s_[:, 0:1], 1.0 / D, EPS, ALU.mult, ALU.add, [f"nst{i}a"], [f"nst{i}b"])
            C.act(s_[:, 2:3], s_[:, 1:2], AF.Sqrt, [f"nst{i}b"], [f"nst{i}c"])
            C.recip(s_[:, 3:4], s_[:, 2:3], [f"nst{i}c"], [f"nst{i}d"])
            C.ts("dve", x_[:, :], x_[:, :], s_[:, 3:4], None, ALU.mult, None, [f"xt{i}", f"nst{i}d"], [f"xt{i}"])
            for g4 in range(4):
                pb = 2 + (g4 % 2)
                pT = PS[pb][:, :].rearrange("p (a b) -> p a b", b=128)
                for a in range(4):
                    kc = g4 * 4 + a
                    C.tr(pT[:, a, :], x_[:, kc * 128:(kc + 1) * 128], ident[:, :], [f"xt{i}", "ident"], [f"ps{pb}"])
                for a in range(4):
                    kc = g4 * 4 + a
                    p.op("dve", lambda e, kc=kc, a=a, pT=pT, h_=h_, lc=lc: e.tensor_scalar(
                        out=h_[:, kc, :], in0=pT[:, a, :], scalar1=Amod[:, s, kc, lc:lc + 1], scalar2=modT[:, (3 * s) * 16 + kc, lc:lc + 1],
                        op0=ALU.mult, op1=ALU.add), [f"ps{pb}", "Amod", "modT"], [hk], nosame=True)
            C.cp("act", hT[:, :, tt * 128:(tt + 1) * 128], h_[:, :, :], [hk], [("hT", tt)])
            for kc in range(16):
                C.mm(PS[7][:, 0:20], h_[:, kc, :], wr[:, kc, :], kc == 0, kc == 15, [hk, "wr"], ["ps7"])
            r_ = rt[i]; rk = f"rt{i}"
            lg = r_[:, 0:20]
            C.tt("dve", lg, PS[7][:, 0:20], br[:, :], ALU.add, ["ps7", "br"], [rk])
            def R(o, x, y, op): C.tt("dve", o, x, y, op, [rk], [rk])
            def RS(o, x, s1, s2, op0, op1): C.ts("dve", o, x, s1, s2, op0, op1, [rk], [rk])
            gmax = r_[:, 20:21]; goh = r_[:, 24:28]; ngmax = r_[:, 21:22]; gsum = r_[:, 22:23]; gw = r_[:, 23:24]
            C.red(gmax, r_[:, 0:4], ALU.max, [rk], [rk])
            RS(goh, r_[:, 0:4], gmax, None, ALU.is_equal, None)
            RS(ngmax, gmax, -1.0, None, ALU.mult, None)
            C.act(r_[:, 28:32], r_[:, 0:4], AF.Exp, [rk], [rk], bias=ngmax, accum=gsum)
            C.recip(gw, gsum, [rk], [rk])
            prod = r_[:, 32:48]
            R(prod.rearrange("p (g j) -> p g j", j=4), r_[:, 4:20].rearrange("p (g j) -> p g j", j=4),
              goh.unsqueeze(2).to_broadcast([128, 4, 4]), ALU.mult)
            ein = r_[:, 48:52]
            C.red(ein, prod.rearrange("p (g j) -> p j g", j=4), ALU.add, [rk], [rk])
            m1 = r_[:, 52:53]; oh1 = r_[:, 56:60]; e2 = r_[:, 60:64]; m2 = r_[:, 53:54]; oh2 = r_[:, 64:68]
            C.red(m1, ein, ALU.max, [rk], [rk])
            RS(oh1, ein, m1, None, ALU.is_equal, None)
            C.stt(e2, oh1, -1e30, ein, ALU.mult, ALU.add, [rk], [rk])
            C.red(m2, e2, ALU.max, [rk], [rk])
            RS(oh2, e2, m2, None, ALU.is_equal, None)
            dm = r_[:, 54:55]; ed = r_[:, 55:56]; w1 = r_[:, 68:69]; w2 = r_[:, 69:70]; dn = r_[:, 70:71]
            R(dm, m2, m1, ALU.subtract)
            C.act(ed, dm, AF.Exp, [rk], [rk])
            RS(dn, ed, 1.0, None, ALU.add, None)
            C.recip(dn, dn, [rk], [rk])
            R(w1, dn, gw, ALU.mult)
            R(w2, ed, w1, ALU.mult)
            c4 = r_[:, 72:76]
            RS(c4, oh1, w1, None, ALU.mult, None)
            C.stt(c4, oh2, w2, c4, ALU.mult, ALU.add, [rk], [rk])
            C.tt("dve", cw[:, tt, :].rearrange("p (g j) -> p g j", j=4), goh.unsqueeze(2).to_broadcast([128, 4, 4]),
                 c4.unsqueeze(1).to_broadcast([128, 4, 4]), ALU.mult, [rk], ["cw"])
        C.dma("sp", CW[:, :, :], cw[:, :, :], "cwout", ["cw"], ["CW"])
        C.dma("sp", HT2[:, :, :], hT[:, :, :], "ht2out", [("hT", t_) for t_ in range(NT)], ["HT2"])
        sb.release(m0)

    def phase_moe(l):
        m0 = sb.mark()
        cw = sb.alloc("cw", [128, NT, 16], F32)
        C.dma("sp", cw[:, :, :], CW[:, :, :], "cwin", ["CW"], ["cw"])
        HALF = 9 * 128
        hTh = sb.alloc("hTh", [128, 16, HALF], BF16)
        acc = sb.alloc("moeacc", [128, 9, 2048], F32)
        hch = [(0, 512), (512, 512), (1024, 128)]
        it = 0; ucnt = 0; dcnt = 0; rcnt = 0
        for half in range(2):
            mE = sb.mark()
            w1b = [sb.alloc(f"w1b{i}", [128, 16, 256], BF16) for i in range(2)]
            w3b = [sb.alloc(f"w3b{i}", [128, 16, 256], BF16) for i in range(2)]
            w2h = [sb.alloc(f"w2h{i}", [128, 4, 2048], BF16) for i in range(2)]
            actT = sb.alloc("actT", [128, 8, HALF], BF16)
            sil = [sb.alloc(f"sil{i}", [128, 512], F32) for i in range(2)]
            C.dma("sp", hTh[:, :, :], HT2[:, :, half * HALF:(half + 1) * HALF], "hTh", ["HT2"], ["hTh"])
            for e_i in range(16):
                def load_up(q, b):
                    p.dma("pool", [lambda e, b=b, q=q, e_i=e_i: e.dma_start(
                        out=w1b[b][:, :, :], in_=I["moe_w1"][l, e_i].rearrange("(k p) n -> p k n", p=128)[:, :, q * 256:(q + 1) * 256])],
                        f"w1b{b}", [], [f"w1b{b}"])
                    p.dma("pool", [lambda e, b=b, q=q, e_i=e_i: e.dma_start(
                        out=w3b[b][:, :, :], in_=I["moe_w3"][l, e_i].rearrange("(k p) n -> p k n", p=128)[:, :, q * 256:(q + 1) * 256])],
                        f"w3b{b}", [], [f"w3b{b}"])
                def load_down():
                    for hh in range(2):
                        p.dma("pool", [lambda e, hh=hh, e_i=e_i: e.dma_start(
                            out=w2h[hh][:, :, :], in_=I["moe_w2"][l, e_i, hh * 512:(hh + 1) * 512, :].rearrange("(k p) n -> p k n", p=128))],
                            f"w2h{hh}", [], [f"w2h{hh}"])
                bq = [(it + q) % 2 for q in range(4)]
                load_up(0, bq[0]); load_up(1, bq[1]); load_down()
                for q in range(4):
                    b = bq[q]
                    if q >= 1 and q + 1 < 4:
                        load_up(q + 1, bq[q + 1])
                    W1, W3 = w1b[b], w3b[b]
                    k1, k3 = f"w1b{b}", f"w3b{b}"
                    for (t0, tw) in hch:
                        for f in range(2):
                            u = ucnt % 2; ucnt += 1
                            for kc in range(16):
                                C.mm(PS[u][:, 0:tw], W1[:, kc, f * 128:(f + 1) * 128], hTh[:, kc, t0:t0 + tw], kc == 0, kc == 15, [k1, "hTh"], [f"ps{u}"])
                            for kc in range(16):
                                C.mm(PS[2 + u][:, 0:tw], W3[:, kc, f * 128:(f + 1) * 128], hTh[:, kc, t0:t0 + tw], kc == 0, kc == 15, [k3, "hTh"], [f"ps{2 + u}"])
                            sl_ = sil[u]; sk_ = f"sil{u}"
                            C.act(sl_[:, 0:tw], PS[u][:, 0:tw], AF.Silu, [f"ps{u}"], [sk_])
                            C.tt("dve", actT[:, q * 2 + f, t0:t0 + tw], PS[2 + u][:, 0:tw], sl_[:, 0:tw], ALU.mult, [f"ps{2 + u}", sk_], ["actT"], )
                it += 4
                first = (e_i == 0)
                for ti in range(9):
                    tg = half * 9 + ti
                    for nb in range(4):
                        pb = 4 + dcnt % 4; dcnt += 1
                        for f in range(8):
                            C.mm(PS[pb][:, :], actT[:, f, ti * 128:(ti + 1) * 128], w2h[f // 4][:, f % 4, nb * 512:(nb + 1) * 512], f == 0, f == 7,
                                 ["actT", f"w2h{f // 4}"], [f"ps{pb}"])
                        dst = acc[:, ti, nb * 512:(nb + 1) * 512]
                        if first:
                            C.ts("dve", dst, PS[pb][:, :], cw[:, tg, e_i:e_i + 1], None, ALU.mult, None, [f"ps{pb}", "cw"], [("moeacc", ti, nb)])
                        else:
                            C.stt(dst, PS[pb][:, :], cw[:, tg, e_i:e_i + 1], dst, ALU.mult, ALU.add, [f"ps{pb}", "cw", ("moeacc", ti, nb)], [("moeacc", ti, nb)])
            sb.release(mE)
            gB = sb.alloc("gateB", [128, 2, 2048], F32)
            build_gateB(gB, 5)
            evs = [sb.alloc(f"rev{i}", [128, 512], F32) for i in range(2)]
            for ti in range(9):
                tg = half * 9 + ti
                for nb in range(4):
                    resid_update(acc[:, ti, nb * 512:(nb + 1) * 512], gB, tg, nb, [("moeacc", ti, nb)], evs, rcnt)
                    rcnt += 1
            sb.release(mE)
        sb.release(m0)

    for l in range(n_layers):
        if stop_after == "init":
            break
        phase_mod(l)
        if stop_after == "mod":
            break
        p.fence()
        m1 = sb.mark()
        hT = sb.alloc("hT", [128, 16, T], BF16)
        phase_norm(l, 0, hT)
        if "HT" in dbg:
            HT = dram_tmp("HT", [128, 16, T], BF16)
            C.dma("sp", HT[:, :, :], hT[:, :, :], "dbg", [("hT", t_) for t_ in range(NT)], ["HT"])
        if stop_after == "norm":
            break
        phase_inproj(l, hT)
        sb.release(m1)
        p.fence()
        if stop_after == "inproj":
            break
        lam_init = 0.8 - 0.6 * math.exp(-0.3 * l)
        if only is None or "s5" in only:
            phase_s5(l)
        if only is None or "win" in only:
            phase_win(l)
        if only is None or "diff" in only:
            phase_diff(l, lam_init)
        if only is None or "na" in only:
            phase_na(l)
        if stop_after == "attn":
            break
        phase_merge(l)
        if stop_after == "merge":
            break
        phase_wout(l)
        if stop_after == "wout":
            break
        m2 = sb.mark()
        hT = sb.alloc("hT", [128, 16, T], BF16)
        phase_router_norm(l, hT)
        sb.release(m2)
        if stop_after == "norm2":
            break
        if only is None or "moe" in only:
            phase_moe(l)

    fin = []
    if stop_after is None:
        p.fence()
        m_f = sb.mark()
        cpf = [sb.alloc(f"cpf{i}", [128, D], F32) for i in range(2)]
        for tt in range(NCT, NT):
            i = tt % 2
            C.dma("sp", cpf[i][:, :], XR[tt * 128:(tt + 1) * 128, :], f"cpb{i}", [("XR", tt)], [f"cpf{i}"])
            C.dma("sp", out[(tt - NCT) * 128:(tt - NCT + 1) * 128, :], cpf[i][:, :], "outw", [f"cpf{i}"], [("out", tt)])
        sb.release(m_f)
        fin.append("outw")
    else:
        C.dma("sp", out[0:128, 0:128], ident[:, :], "outw", ["ident"], [("out", 0)])
        fin.append("outw")
    if "dbg" in p.dsem:
        fin.append("dbg")
    for k in ("pev0", "pev1", "sgv0", "sgv1"):
        if k in p.dsem:
            fin.append(k)
    p.emit(final_sems=fin)
    return nc


SEQ_ = 2048


def prep_inputs(inp, b, nl=DEPTH):
    m = {}
    m["x"] = np.ascontiguousarray(inp["x"][b])
    m["ctx"] = np.ascontiguousarray(inp["ctx"][b])
    cT = np.stack([inp["c"][b].reshape(16, 128).T, inp["c_ctx"].reshape(16, 128).T], axis=-1)
    m["cT"] = np.ascontiguousarray(cT.astype(np.float32))
    m["w_ada"] = inp["w_ada"][:nl]
    m["b_adaT"] = np.ascontiguousarray(inp["b_ada"].reshape(-1, 96, 128).transpose(0, 2, 1)[:nl])
    nm = np.stack([inp["norm_mix"], inp["norm_ffn"]], axis=1)
    m["normT"] = np.ascontiguousarray(nm.reshape(-1, 2, 16, 128).transpose(0, 1, 3, 2)[:nl])
    m["w_in"] = inp["w_in"][:nl]
    m.update(host_consts())
    m["qkg"] = np.ascontiguousarray(np.concatenate([inp[k][:nl] for k in ("win_qn", "win_kn", "diff_qn", "diff_kn", "na_qn", "na_kn")], axis=1))
    m["sink"] = np.ascontiguousarray(inp["win_sink"][:nl])
    m["dlam"] = np.ascontiguousarray(inp["diff_lambda"][:nl].reshape(nl, 256))
    m["subg"] = np.ascontiguousarray(inp["diff_subln"][:nl])
    m["nab"] = na_bias_layout(inp["na_rpb"][:nl])
    def dp(a):
        a = np.moveaxis(a[:nl], 3, 2)
        return a.reshape((nl, 128) + a.shape[3:])
    m["s5a"] = np.ascontiguousarray(np.stack([dp(inp["s5_a_re"]), dp(inp["s5_a_im"])], axis=-1))
    m["s5ls"] = np.ascontiguousarray(np.repeat(inp["s5_log_step"][:nl][:, :, None, :], 64, axis=2).reshape(nl, 128, 32))
    m["s5b"] = np.ascontiguousarray(np.stack([dp(inp["s5_b_re"]), dp(inp["s5_b_im"])], axis=3))
    cre = np.swapaxes(inp["s5_c_re"], 3, 4); cim = np.swapaxes(inp["s5_c_im"], 3, 4)
    m["s5c"] = np.ascontiguousarray(np.stack([dp(cre), dp(cim)], axis=3))
    dg = inp["s5_d"][:nl].reshape(nl, 32, 16)
    m["s5d"] = np.ascontiguousarray(np.tile(dg.transpose(0, 2, 1)[:, None, :, :], (1, 8, 1, 1)).reshape(nl, 128, 32))
    m["s5_w_glu"] = inp["s5_w_glu"][:nl]
    m["s5_b_glu"] = inp["s5_b_glu"][:nl]
    m["w_branch"] = np.ascontiguousarray(inp["w_branch"][:nl].reshape(nl, 2048, 2048))
    m["w_out"] = inp["w_out"][:nl]
    wr = np.concatenate([inp["moe_w_group"][:nl], inp["moe_w_expert"][:nl]], axis=2)
    m["wrT"] = np.ascontiguousarray(wr.reshape(nl, 16, 128, 20).transpose(0, 2, 1, 3).reshape(nl, 128, 320))
    m["brt"] = np.ascontiguousarray(np.concatenate([inp["moe_b_group"][:nl], inp["moe_b_expert"][:nl]], axis=1))
    m["moe_w1"] = inp["moe_w1"][:nl]
    m["moe_w3"] = inp["moe_w3"][:nl]
    m["moe_w2"] = inp["moe_w2"][:nl]
    return m


_CONSTS = {}


def host_consts():
    if not _CONSTS:
        t = np.arange(2048)
        row = (t // 64).astype(np.float32); col = (t % 64).astype(np.float32)
        inv = (100.0 ** (-np.arange(16, dtype=np.float32) / 16)).astype(np.float32)
        ang = np.concatenate([row[:, None] * inv, col[:, None] * inv], axis=-1).astype(np.float32)
        cs = np.stack([np.cos(ang), np.sin(ang)], axis=1).astype(np.float32)
        _CONSTS["rope"] = np.ascontiguousarray(cs.reshape(16, 128, 2, 32).transpose(1, 0, 2, 3))
        j = np.arange(128)[:, None]; i = np.arange(128)[None, :]
        mL = (i <= j).astype(np.float32); mR = (j <= i).astype(np.float32)
        _CONSTS["wmask"] = np.ascontiguousarray(np.stack([np.tile(mL, (1, 4)), np.tile(mR, (1, 4))], axis=0))
        tau = np.arange(128)[:, None] // 16; ii = np.arange(128)[None, :] // 16
        _CONSTS["s5mask"] = np.ascontiguousarray(np.stack([(tau <= ii), (tau >= ii)], axis=0).astype(np.float32))
    return dict(_CONSTS)


def na_bias_layout(rpb):
    nl = rpb.shape[0]
    out = np.full((nl, 5, 128, 8, 5, 128), NEG, np.float32)
    rep_n = [0, 1, 5, 14, 15]
    qi = np.arange(128); qr_l = qi // 64; qc = qi % 64
    ki = np.arange(128); kr_l = ki // 64; kc = ki % 64
    cstart = np.clip(qc - 8, 0, 48)
    col_ok = (kc[None, :] >= cstart[:, None]) & (kc[None, :] < cstart[:, None] + 16)
    c_off = np.clip(kc[None, :] - qc[:, None] + 15, 0, 30)
    for ty, n in enumerate(rep_n):
        r0 = 2 * n
        s0 = min(max(r0 - 4, 0), 24); s1 = min(max(r0 + 1 - 4, 0), 24)
        first = s0 // 2; last = (s1 + 7) // 2
        for sl, kt in enumerate(range(first, last + 1)):
            qrow = r0 + qr_l
            krow = 2 * kt + kr_l
            srow = np.clip(qrow - 4, 0, 24)
            row_ok = (krow[None, :] >= srow[:, None]) & (krow[None, :] < srow[:, None] + 8)
            ok = row_ok & col_ok
            r_off = np.clip(krow[None, :] - qrow[:, None] + 7, 0, 14)
            g = rpb[:, :, r_off, c_off]
            g = np.where(ok[None, None], g, np.float32(NEG))
            out[:, ty, :, :, sl, :] = g.transpose(0, 2, 1, 3)
    return np.ascontiguousarray(out.reshape(nl, 5, 128, 8 * 5 * 128))


_NC_CACHE = {}


def kernel(**inputs):
    inp = {k: np.asarray(v) for k, v in inputs.items()}
    if "full" not in _NC_CACHE:
        _NC_CACHE["full"] = build_program()
    nc = _NC_CACHE["full"]
    in_maps = [prep_inputs(inp, b) for b in range(8)]
    res = run_bass_kernel_spmd(nc, in_maps, core_ids=list(range(8)))
    return np.stack([np.asarray(r["out"]) for r in res.results], axis=0).astype(np.float32)
```
